# Optimizing a Trainium2 kernel written in Bass

```python
import jax, jax.numpy as jnp
from jax import lax
import numpy as np

D_MODEL = 2048
BATCH = 8
SEQ = 2048
DEPTH = 2

HEAD_DIM = 128
DIL_CONFIGS = ((128, 1), (512, 4), (2048, 16))
N_DIL_GROUPS = 3
HEADS_PER_DIL_GROUP = 4
N_SB_HEADS = 8
SB_BLOCK = 128
ROPE_THETA = 500000.0
ROPE_DIM = HEAD_DIM // 4
N_EXPERT_GROUPS = 4
EXPERTS_PER_GROUP = 4
N_EXPERTS = N_EXPERT_GROUPS * EXPERTS_PER_GROUP
TOP_K_FINE = 2
EXPERT_FF = D_MODEL // 4
LN_EPS = 1e-5
DEEPNORM_ALPHA = (2 * DEPTH) ** 0.25
DEEPNORM_BETA = (8 * DEPTH) ** -0.25

WIDTH_A = N_DIL_GROUPS * HEADS_PER_DIL_GROUP * HEAD_DIM
OUT_A = HEADS_PER_DIL_GROUP * HEAD_DIM
WIDTH_B = N_SB_HEADS * HEAD_DIM
IN_SPLITS = (WIDTH_A, WIDTH_A, WIDTH_A, WIDTH_B, WIDTH_B, WIDTH_B, D_MODEL, D_MODEL)
IN_WIDTH = 3 * WIDTH_A + 3 * WIDTH_B + 2 * D_MODEL

kernel_name = "dilated_stickbreak_hier_moe_deepnorm"


def layer_norm(x, g, b):
    xf = x.astype(jnp.float32)
    mu = jnp.mean(xf, axis=-1, keepdims=True)
    var = jnp.mean(jnp.square(xf - mu), axis=-1, keepdims=True)
    return ((xf - mu) * lax.rsqrt(var + LN_EPS) * g + b).astype(x.dtype)


def partial_rotary(t, positions):
    half = ROPE_DIM // 2
    inv_freq = ROPE_THETA ** (-2.0 * jnp.arange(half, dtype=jnp.float32) / ROPE_DIM)
    ang = positions.astype(jnp.float32)[:, :, None] * inv_freq
    cos = jnp.cos(ang)[:, :, None, None, :]
    sin = jnp.sin(ang)[:, :, None, None, :]
    tf = t.astype(jnp.float32)
    x1, x2 = tf[..., :half], tf[..., half:ROPE_DIM]
    rot = jnp.concatenate([x1 * cos - x2 * sin, x2 * cos + x1 * sin, tf[..., ROPE_DIM:]], axis=-1)
    return rot.astype(t.dtype)


def dilated_window_attention(q, k, v, window, dilation):
    B, S, H, Dh = q.shape
    n = window // dilation
    sub_len = -(-S // dilation)
    sub_len = -(-sub_len // n) * n
    s_pad = sub_len * dilation
    nb = sub_len // n

    def to_blocks(t):
        t = jnp.pad(t, ((0, 0), (0, s_pad - S), (0, 0), (0, 0)))
        t = t.reshape(B, sub_len, dilation, H, Dh).transpose(0, 2, 3, 1, 4)
        return t.reshape(B, dilation, H, nb, n, Dh)

    def with_prev(t):
        prev = jnp.concatenate([jnp.zeros_like(t[:, :, :, :1]), t[:, :, :, :-1]], axis=3)
        return jnp.concatenate([prev, t], axis=4)

    qb = to_blocks(q)
    kk = with_prev(to_blocks(k))
    vv = with_prev(to_blocks(v))
    s = jnp.einsum('brhnqd,brhnkd->brhnqk', qb, kk).astype(jnp.float32) * (Dh ** -0.5)
    qi = jnp.arange(n)[:, None]
    kj = jnp.arange(2 * n)[None, :]
    band = (kj >= qi) & (kj <= qi + n)
    in_range = (jnp.arange(nb)[:, None, None] > 0) | (kj >= n)[None]
    valid = band[None] & in_range
    s = jnp.where(valid, s, -jnp.inf)
    m = jnp.max(s, axis=-1, keepdims=True)
    p = jnp.exp(s - m)
    den = jnp.sum(p, axis=-1, keepdims=True)
    out = jnp.einsum('brhnqk,brhnkd->brhnqd', (p / den).astype(v.dtype), vv)
    lse = (m + jnp.log(den))[..., 0]
    out = out.reshape(B, dilation, H, sub_len, Dh).transpose(0, 3, 1, 2, 4).reshape(B, s_pad, H, Dh)[:, :S]
    lse = lse.reshape(B, dilation, H, sub_len).transpose(0, 3, 1, 2).reshape(B, s_pad, H)[:, :S]
    return out, lse


def stick_breaking_attention(q, k, v):
    B, S, H, Dh = q.shape
    outs = []
    for i in range(S // SB_BLOCK):
        q0 = i * SB_BLOCK
        kv_len = q0 + SB_BLOCK
        z = jnp.einsum('bqhd,bkhd->bhqk', q[:, q0:kv_len], k[:, :kv_len]).astype(jnp.float32) * (Dh ** -0.5)
        t_idx = q0 + jnp.arange(SB_BLOCK)
        causal = jnp.arange(kv_len)[None, :] < t_idx[:, None]
        log_keep = jnp.where(causal, jax.nn.log_sigmoid(-z), 0.0)
        later = lax.cumsum(log_keep, axis=3, reverse=True) - log_keep
        log_a = jnp.where(causal, jax.nn.log_sigmoid(z) + later, -jnp.inf)
        outs.append(jnp.einsum('bhqk,bkhd->bqhd', jnp.exp(log_a).astype(v.dtype), v[:, :kv_len]))
    return jnp.concatenate(outs, axis=1)


def hybrid_mixer(x, positions, w_in, p_a, p_b, w_o):
    B, S, _ = x.shape
    proj = jnp.einsum('bsd,de->bse', x, w_in)
    qa, ka, va, qb, kb, vb, ga, gb = jnp.split(proj, np.cumsum(IN_SPLITS)[:-1], axis=-1)
    shp_a = (B, S, N_DIL_GROUPS, HEADS_PER_DIL_GROUP, HEAD_DIM)
    qa = partial_rotary(qa.reshape(shp_a), positions)
    ka = partial_rotary(ka.reshape(shp_a), positions)
    va = va.reshape(shp_a)
    outs, lses = [], []
    for g, (window, dilation) in enumerate(DIL_CONFIGS):
        o, l = dilated_window_attention(qa[:, :, g], ka[:, :, g], va[:, :, g], window, dilation)
        outs.append(o)
        lses.append(l)
    w_grp = jax.nn.softmax(jnp.stack(lses, axis=0), axis=0)
    o_a = jnp.sum(w_grp[..., None].astype(x.dtype) * jnp.stack(outs, axis=0), axis=0).reshape(B, S, OUT_A)
    shp_b = (B, S, N_SB_HEADS, HEAD_DIM)
    o_b = stick_breaking_attention(qb.reshape(shp_b), kb.reshape(shp_b), vb.reshape(shp_b)).reshape(B, S, WIDTH_B)
    merged = (jax.nn.sigmoid(ga) * jnp.einsum('bse,ed->bsd', o_a, p_a)
              + jax.nn.sigmoid(gb) * jnp.einsum('bse,ed->bsd', o_b, p_b))
    return jnp.einsum('bsd,de->bse', merged, w_o).astype(x.dtype)


def hierarchical_moe(x, w_coarse, b_coarse, w_fine, b_fine, w_up, w_down):
    B, S, D = x.shape
    xt = x.reshape(B * S, D)
    coarse_p = jax.nn.softmax((xt @ w_coarse).astype(jnp.float32) + b_coarse, axis=-1)
    p_grp, g_idx = lax.top_k(coarse_p, 1)
    fine_logits = ((xt @ w_fine).astype(jnp.float32) + b_fine).reshape(-1, N_EXPERT_GROUPS, EXPERTS_PER_GROUP)
    sel = jnp.take_along_axis(fine_logits, g_idx[:, :, None], axis=1)[:, 0]
    top_v, top_i = lax.top_k(sel, TOP_K_FINE)
    w_top = jax.nn.softmax(top_v, axis=-1)
    within = jnp.sum(jax.nn.one_hot(top_i, EXPERTS_PER_GROUP, dtype=jnp.float32) * w_top[..., None], axis=1)
    gates = (jax.nn.one_hot(g_idx[:, 0], N_EXPERT_GROUPS, dtype=jnp.float32)[:, :, None]
             * (p_grp * within)[:, None, :]).reshape(-1, N_EXPERTS)
    y = jnp.zeros((B * S, D), jnp.float32)
    for e in range(N_EXPERTS):
        h = xt @ w_up[e]
        a, b = h[:, :EXPERT_FF], h[:, EXPERT_FF:]
        y = y + gates[:, e:e + 1] * ((jax.nn.silu(a) * b) @ w_down[e]).astype(jnp.float32)
    return y.reshape(B, S, D).astype(x.dtype)


def setup_inputs(seed: int = 0) -> dict:
    key = jax.random.key(seed)
    ks = jax.random.split(key, 16)
    nrm = jax.random.normal
    x = nrm(ks[0], (BATCH, SEQ, D_MODEL), jnp.float32)
    offs = jax.random.randint(ks[1], (BATCH, 1), 0, 4096, dtype=jnp.int32)
    positions = (jnp.arange(SEQ, dtype=jnp.int32)[None, :] + offs).astype(jnp.int32)
    w_in = nrm(ks[2], (DEPTH, D_MODEL, IN_WIDTH), jnp.float32) * D_MODEL ** -0.5
    p_a = nrm(ks[3], (DEPTH, OUT_A, D_MODEL), jnp.float32) * OUT_A ** -0.5
    p_b = nrm(ks[4], (DEPTH, WIDTH_B, D_MODEL), jnp.float32) * WIDTH_B ** -0.5
    w_o = nrm(ks[5], (DEPTH, D_MODEL, D_MODEL), jnp.float32) * (D_MODEL ** -0.5 * DEEPNORM_BETA)
    ln_g = 1.0 + 0.02 * nrm(ks[6], (DEPTH, 2, D_MODEL), jnp.float32)
    ln_b = 0.02 * nrm(ks[7], (DEPTH, 2, D_MODEL), jnp.float32)
    router_coarse = nrm(ks[8], (DEPTH, D_MODEL, N_EXPERT_GROUPS), jnp.float32) * D_MODEL ** -0.5
    router_coarse_bias = 0.01 * nrm(ks[9], (DEPTH, N_EXPERT_GROUPS), jnp.float32)
    router_fine = nrm(ks[10], (DEPTH, D_MODEL, N_EXPERTS), jnp.float32) * D_MODEL ** -0.5
    router_fine_bias = 0.01 * nrm(ks[11], (DEPTH, N_EXPERTS), jnp.float32)
    w_up = nrm(ks[12], (DEPTH, N_EXPERTS, D_MODEL, 2 * EXPERT_FF), jnp.float32) * D_MODEL ** -0.5
    w_down = nrm(ks[13], (DEPTH, N_EXPERTS, EXPERT_FF, D_MODEL), jnp.float32) * (EXPERT_FF ** -0.5 * DEEPNORM_BETA)
    return {"x": x, "positions": positions, "w_in": w_in, "p_a": p_a, "p_b": p_b, "w_o": w_o,
            "ln_g": ln_g, "ln_b": ln_b, "router_coarse": router_coarse,
            "router_coarse_bias": router_coarse_bias, "router_fine": router_fine,
            "router_fine_bias": router_fine_bias, "w_up": w_up, "w_down": w_down}


def reference(x, positions, w_in, p_a, p_b, w_o, ln_g, ln_b, router_coarse, router_coarse_bias,
              router_fine, router_fine_bias, w_up, w_down):
    h = x
    for l in range(DEPTH):
        y = hybrid_mixer(h, positions, w_in[l], p_a[l], p_b[l], w_o[l])
        h = layer_norm(DEEPNORM_ALPHA * h + y, ln_g[l, 0], ln_b[l, 0])
        y = hierarchical_moe(h, router_coarse[l], router_coarse_bias[l], router_fine[l],
                             router_fine_bias[l], w_up[l], w_down[l])
        h = layer_norm(DEEPNORM_ALPHA * h + y, ln_g[l, 1], ln_b[l, 1])
    return h
```

```python
import numpy as np
import concourse.bass as bass
import concourse.mybir as mybir
from concourse.bass_utils import run_bass_kernel_spmd

F32 = mybir.dt.float32
BF16 = mybir.dt.bfloat16
I32 = mybir.dt.int32
AF = mybir.ActivationFunctionType
ALU = mybir.AluOpType
AX = mybir.AxisListType

ENGINES = ("sync", "scalar", "gpsimd", "vector", "tensor")

S = 2048
D = 2048
KC = 16
NT = 16
L = 2
NCORES = 8
ALPHA = float((2 * L) ** 0.25)
LN_EPS = 1e-5
SCALE = float(128 ** -0.5)
DILS = (1, 4, 16)
PI = float(np.pi)
NCONST = 128 * 8
SPARSE_MOE = True


class Prog:
    def __init__(self, nc):
        self.nc = nc
        self.q = {e: [] for e in ENGINES}
        self.cnt = {}
        self.sems = {}
        self.waited = {}
        self._ctx = []
        self.base = []
        for e in ENGINES:
            self._sem("E_" + e)

    def _sem(self, name):
        if name not in self.sems:
            cm = self.nc.semaphore(name)
            self.sems[name] = cm.__enter__()
            self.cnt[name] = 0
        return self.sems[name]

    def sbuf(self, name, shape, dt):
        self._uid = getattr(self, "_uid", 0) + 1
        cm = self.nc.sbuf_tensor(f"sb{self._uid}_{name}", list(shape), dt)
        t = cm.__enter__()
        self._ctx.append(cm)
        return t

    def psum(self, name, shape, dt):
        cm = self.nc.psum_tensor(name, list(shape), dt)
        t = cm.__enter__()
        self._ctx.append(cm)
        return t

    def _flat(self, deps, out):
        for tok in deps:
            if tok is None:
                continue
            if isinstance(tok, tuple) and len(tok) == 2 and isinstance(tok[0], str):
                out.append(tok)
            else:
                self._flat(tok, out)

    def _waits(self, eng, deps):
        flat = []
        self._flat(deps, flat)
        self._flat(self.base, flat)
        mx = {}
        for sname, val in flat:
            if mx.get(sname, 0) < val:
                mx[sname] = val
        waits = []
        for sname, val in mx.items():
            key = (eng, sname)
            if self.waited.get(key, 0) < val:
                self.waited[key] = val
                waits.append((sname, val))
        return waits

    def op(self, eng, fn, deps=(), sig=True):
        waits = self._waits(eng, deps)
        tok = None
        sname = None
        if sig:
            sname = "E_" + eng
            self.cnt[sname] += 1
            tok = (sname, self.cnt[sname])
        self.q[eng].append((fn, waits, sname, 1))
        return tok

    def dma(self, eng, out, in_, deps=(), stream=None):
        waits = self._waits(eng, deps)
        sname = "D_" + (stream or eng)
        self._sem(sname)
        self.cnt[sname] += 16
        tok = (sname, self.cnt[sname])
        self.q[eng].append((lambda e: e.dma_start(out=out, in_=in_), waits, sname, 16))
        return tok

    def dma_fn(self, eng, fn, deps=(), stream=None):
        waits = self._waits(eng, deps)
        sname = "D_" + (stream or eng)
        self._sem(sname)
        self.cnt[sname] += 16
        tok = (sname, self.cnt[sname])
        self.q[eng].append((fn, waits, sname, 16))
        return tok

    def finish_wait(self, eng, deps):
        waits = self._waits(eng, deps)
        self.q[eng].append((None, waits, None, 0))

    def barrier(self):
        return [(n, c) for n, c in self.cnt.items() if c > 0]

    class _Phase:
        def __init__(self, P):
            self.P = P

        def __enter__(self):
            self.depth = len(self.P._ctx)
            self.P.base = self.P.barrier()
            return self

        def __exit__(self, *a):
            P = self.P
            while len(P._ctx) > self.depth:
                P._ctx.pop().__exit__(None, None, None)
            P.base = P.barrier()
            return False

    def phase(self):
        return Prog._Phase(self)

    def emit(self):
        nc = self.nc
        with nc.Block() as block:
            def mk(ename):
                def body(e):
                    for fn, waits, sname, inc in self.q[ename]:
                        for (wn, wv) in waits:
                            e.wait_ge(self.sems[wn], wv)
                        if fn is None:
                            continue
                        ins = fn(e)
                        if sname is not None:
                            ins.then_inc(self.sems[sname], inc)
                return body
            block.sync(mk("sync"))
            block.scalar(mk("scalar"))
            block.gpsimd(mk("gpsimd"))
            block.vector(mk("vector"))
            block.tensor(mk("tensor"))

    def close(self):
        while self._ctx:
            self._ctx.pop().__exit__(None, None, None)


class Ring:
    def __init__(self, bufs):
        self.bufs = list(bufs)
        self.i = 0
        self.rel = [[] for _ in self.bufs]

    def get(self):
        idx = self.i % len(self.bufs)
        self.i += 1
        return idx, self.bufs[idx], self.rel[idx]

    def release(self, idx, toks):
        self.rel[idx] = list(toks)


def build_nc(n_layers=L, stop_after=None, dbg=()):
    nc = bass.Bass("TRN2", target_bir_lowering=False)

    def din(name, shape, dt=F32):
        return nc.dram_tensor(name, list(shape), dt, kind="ExternalInput").ap()

    x_d = din("x", [S, D])
    pos_d = din("pos", [32, S], I32)
    win_d = din("w_in", [L, 92, 128, KC, 128])
    pa_d = din("p_a", [L, 16, 128, 4, 128])
    pb_d = din("p_b", [L, 16, 128, 8, 128])
    wo_d = din("w_o", [L, 128, KC, D])
    lng_d = din("ln_gb", [L, 2, 2, 128, D])
    wr_d = din("w_r", [L, 128, KC, 20])
    rb_d = din("r_b", [L, 128, 20])
    wup_d = din("w_up", [L, 16 * 4 * 2 * 128, 2048])
    wdn_d = din("w_down", [L, 16 * 512, D])
    KT = 32
    TS = 256
    XS_d = nc.dram_tensor("xs_scr", [KT * TS, D], BF16).ap()
    YS_d = nc.dram_tensor("ys_scr", [KT * TS, D], F32).ap()
    cst_d = din("consts", [128, NCONST])
    csc_d = din("cscal", [128, 64])
    out_d = nc.dram_tensor("out", [S, D], F32, kind="ExternalOutput").ap()
    R_d = [nc.dram_tensor(f"resid{i}", [S, D], F32).ap() for i in range(3)]
    mT_d = nc.dram_tensor("mT_scr", [NT, 128, KC, 128], BF16).ap()
    dbg_d = {}
    for name, shape in dbg:
        dbg_d[name] = nc.dram_tensor("dbg_" + name, list(shape), F32, kind="ExternalOutput").ap()

    P = Prog(nc)
    out_toks = []

    pall = P.psum("pall", [128, 8 * 512], F32)
    pbk = [pall[:, i * 512:(i + 1) * 512] for i in range(8)]
    cbf = P.sbuf("cbf", [128, NCONST], BF16)
    ident = P.sbuf("ident", [128, 128], F32)
    cscal = P.sbuf("cscal", [128, 64], F32)
    gates = P.sbuf("gates", [128, NT, 16], F32)
    selmask = P.sbuf("selmask", [128, NT, 16], F32)
    xT = P.sbuf("xT", [128, KC, S], BF16)
    M2 = cbf[:, 0:256]
    MSTRICT = cbf[:, 256:384]
    NEGU = cbf[:, 384:512]
    NEGONES = cbf[:, 512:640]
    ONES = cbf[:, 640:768]
    PERM = cbf[0:32, 768:800]

    t_c1 = P.dma("gpsimd", cbf[:], cst_d, stream="c1")
    t_c2 = P.dma("sync", ident[:], cst_d[:, 896:1024], stream="c2")
    t_c3 = P.dma("sync", cscal[:], csc_d, stream="c3")
    cdeps = [t_c1, t_c2, t_c3]

    def dbg_dump(name, src_ap, dst=None):
        if name in dbg_d:
            t = P.dma("gpsimd", dbg_d[name] if dst is None else dst, src_ap, deps=P.barrier(), stream="dbg")
            out_toks.append(t)

    rot_d = nc.dram_tensor("rot_scr", [2, 32, S], F32).ap()
    rot_tabs = {}
    with P.phase():
        cos_t = P.sbuf("cos_t0", [32, S], F32)
        sin_t = P.sbuf("sin_t0", [32, S], F32)
        posi = P.sbuf("posi", [32, S], I32)
        ang = P.sbuf("ang", [32, S], F32)
        twopi = P.sbuf("twopi", [32, S], F32)
        tmpa = P.sbuf("tmpa", [32, S], F32)
        t_p = P.dma("sync", posi[:], pos_d, stream="pos")
        t0 = P.op("vector", lambda e: e.tensor_copy(ang[:], posi[:]), deps=[t_p])
        t1 = P.op("vector", lambda e: e.tensor_scalar(ang[:], ang[:], cscal[0:32, 0:1], None, ALU.mult), deps=[t0] + cdeps)
        ki = P.sbuf("rot_ki", [32, S], I32)

        def sin_of(dst, shift, dep):
            d = P.op("vector", lambda e: e.tensor_scalar(tmpa[:], ang[:], shift, 1.0 / (2.0 * PI), ALU.add, ALU.mult), deps=[dep])
            d = P.op("vector", lambda e: e.tensor_copy(ki[:], tmpa[:]), deps=[d])
            d = P.op("vector", lambda e: e.tensor_copy(twopi[:], ki[:]), deps=[d])
            d = P.op("vector", lambda e: e.tensor_scalar(tmpa[:], ang[:], shift, None, ALU.add), deps=[d])
            d = P.op("vector", lambda e: e.scalar_tensor_tensor(tmpa[:], twopi[:], -2.0 * PI, tmpa[:], ALU.mult, ALU.add), deps=[d])
            d = P.op("vector", lambda e: e.tensor_scalar(twopi[:], tmpa[:], PI, None, ALU.is_gt), deps=[d])
            d = P.op("vector", lambda e: e.scalar_tensor_tensor(tmpa[:], twopi[:], -2.0 * PI, tmpa[:], ALU.mult, ALU.add), deps=[d])
            d = P.op("vector", lambda e: e.tensor_scalar(twopi[:], tmpa[:], -PI, None, ALU.is_lt), deps=[d])
            d = P.op("vector", lambda e: e.scalar_tensor_tensor(tmpa[:], twopi[:], 2.0 * PI, tmpa[:], ALU.mult, ALU.add), deps=[d])
            d = P.op("vector", lambda e: e.tensor_scalar(tmpa[:], tmpa[:], PI, -PI, ALU.min, ALU.max), deps=[d])
            d = P.op("scalar", lambda e: e.activation(dst[:], tmpa[:], AF.Sin), deps=[d])
            return d

        t4 = sin_of(sin_t, 0.0, t1)
        t5 = P.op("vector", lambda e: e.tensor_scalar(sin_t[:], sin_t[:], cscal[0:32, 1:2], None, ALU.mult), deps=[t4])
        t8 = sin_of(cos_t, PI / 2, t4)
        P.dma("sync", rot_d[0], cos_t[:], deps=[t8], stream="rot")
        P.dma("sync", rot_d[1], sin_t[:], deps=[t5], stream="rot")

    def phase_prep(src_d, l, router):
        with P.phase():
            xs = [P.sbuf(f"xs{i}", [128, D], F32) for i in range(2)]
            ring = Ring(xs)
            pring = Ring([0, 1, 2, 3])
            if router:
                wr = P.sbuf("wr", [128, KC, 20], F32)
                rb = P.sbuf("rb", [128, 20], F32)
                h32s = [P.sbuf(f"h32_{i}", [128, KC, 128], F32) for i in range(2)]
                hring = Ring(h32s)
                LG = P.sbuf("LG", [128, NT, 20], F32)
                lg_toks = []
                t_wr = P.dma("sync", wr[:], wr_d[l], stream="wr")
                t_rb = P.dma("sync", rb[:], rb_d[l], stream="rb")
                lring = Ring([4, 5])
            if not router:
                xb16 = [P.sbuf(f"xb16_{i}", [128, D], BF16) for i in range(2)]
                b16ring = Ring(xb16)
                IDB_ = cbf[:, 896:1024]
            for tt in range(NT):
                i, buf, rel = ring.get()
                t_ld = P.dma("sync", buf[:], src_d[tt * 128:(tt + 1) * 128, :], deps=rel, stream=f"xs{i}")
                if not router:
                    bi_, b16, brel_ = b16ring.get()
                    if tt % 2 == 0:
                        tc_ = P.op("scalar", lambda e, b16=b16, buf=buf: e.copy(b16[:], buf[:]), deps=[t_ld, brel_])
                    else:
                        tc_ = P.op("vector", lambda e, b16=b16, buf=buf: e.tensor_copy(b16[:], buf[:]), deps=[t_ld, brel_])
                    ring.release(i, [tc_])
                    tk = None
                    for g in range(2):
                        pi_, bk, prel = pring.get()
                        bview = pbk[bk].bitcast(BF16)
                        for j in range(8):
                            kc = g * 8 + j
                            tk = P.op("tensor", lambda e, bview=bview, j=j, kc=kc, b16=b16: e.transpose(
                                bview[:, j * 128:(j + 1) * 128], b16[:, kc * 128:(kc + 1) * 128], IDB_),
                                deps=[tc_, prel] + cdeps, sig=(j == 7))
                        src_v = bview.rearrange("p (a b) -> p a b", a=8)
                        dst_v = xT[:, g * 8:(g + 1) * 8, tt * 128:(tt + 1) * 128]
                        if (g + tt) % 2 == 0:
                            te = P.op("vector", lambda e, d=dst_v, s=src_v: e.tensor_copy(d, s), deps=[tk])
                        else:
                            te = P.op("scalar", lambda e, d=dst_v, s=src_v: e.copy(d, s), deps=[tk])
                        pring.release(pi_, [te])
                    b16ring.release(bi_, [tk])
                    continue
                if router:
                    hi, h32, hrel = hring.get()
                tks = []
                h32w = []
                for g in range(4):
                    pi_, bk, prel = pring.get()
                    pb_ = pbk[bk]
                    tk = None
                    for j in range(4):
                        kc = g * 4 + j
                        tk = P.op("tensor", lambda e, pb_=pb_, j=j, kc=kc, buf=buf: e.transpose(
                            pb_[:, j * 128:(j + 1) * 128], buf[:, kc * 128:(kc + 1) * 128], ident[:]),
                            deps=[t_ld, prel] + cdeps, sig=(j == 3))
                    tks.append(tk)
                    src_v = pb_.rearrange("p (a b) -> p a b", a=4)
                    dst_v = xT[:, g * 4:(g + 1) * 4, tt * 128:(tt + 1) * 128]
                    if not router:
                        if g % 2 == 0:
                            te = P.op("scalar", lambda e, d=dst_v, s=src_v: e.copy(d, s), deps=[tk])
                        else:
                            te = P.op("vector", lambda e, d=dst_v, s=src_v: e.tensor_copy(d, s), deps=[tk])
                        rels = [te]
                    else:
                        d2 = h32[:, g * 4:(g + 1) * 4, :]
                        if g % 2 == 0:
                            te2 = P.op("vector", lambda e, d=d2, s=src_v: e.tensor_copy(d, s), deps=[tk, hrel])
                        else:
                            te2 = P.op("scalar", lambda e, d=d2, s=src_v: e.copy(d, s), deps=[tk, hrel])
                        rels = [te2]
                        h32w.append(te2)
                        if not SPARSE_MOE:
                            te = P.op("gpsimd", lambda e, d=dst_v, s=d2: e.tensor_copy(d, s), deps=[te2])
                            h32w.append(te)
                    pring.release(pi_, rels)
                ring.release(i, tks)
                if router:
                    li, lbk, lrel = lring.get()
                    lp = pbk[lbk][:, 0:20]
                    tm = None
                    for kc in range(KC):
                        tm = P.op("tensor", lambda e, lp=lp, h32=h32, kc=kc: e.matmul(
                            lp, h32[:, kc, :], wr[:, kc, :], start=(kc == 0), stop=(kc == KC - 1)),
                            deps=[h32w, t_wr, lrel], sig=(kc == KC - 1))
                    hring.release(hi, [tm] + h32w)
                    ta_ = P.op("vector", lambda e, lp=lp, tt=tt: e.tensor_tensor(LG[:, tt, :], lp, rb[:], ALU.add), deps=[tm, t_rb])
                    lring.release(li, [ta_])
                    lg_toks.append(ta_)
            if router:
                V = "vector"
                T16 = NT

                def W(name, n):
                    return P.sbuf("g_" + name, [128, T16 * n], F32)

                def v3(t, n):
                    return t[:].rearrange("p (t n) -> p t n", t=T16)

                def bc(t, n):
                    return t[:].rearrange("p (t o) -> p t o", o=1).broadcast_to([128, T16, n])

                mx, dd, ex, sme, pg, oh4 = W("mx", 1), W("dd", 4), W("ex", 4), W("sme", 1), W("pg", 1), W("oh4", 4)
                prod, sel, m1, d1, oh1, sel2 = W("prod", 16), W("sel", 4), W("m1", 1), W("d1", 4), W("oh1", 4), W("sel2", 4)
                m2, d2, oh2, dm, ed, w1, w2, wi1, wi2 = W("m2", 1), W("d2", 4), W("oh2", 4), W("dm", 1), W("ed", 1), W("w1", 1), W("w2", 1), W("wi1", 4), W("wi2", 4)
                lc = LG[:, :, 0:4]
                lf = LG[:, :, 4:20]
                c = P.op(V, lambda e: e.reduce_max(mx[:], lc, AX.X), deps=[lg_toks])
                c = P.op(V, lambda e: e.tensor_tensor(v3(dd, 4), lc, bc(mx, 4), ALU.subtract), deps=[c])
                cx = P.op("scalar", lambda e: e.activation(ex[:], dd[:], AF.Exp), deps=[c])
                cx = P.op(V, lambda e: e.reduce_sum(sme[:], v3(ex, 4), AX.X), deps=[cx])
                cx = P.op(V, lambda e: e.reciprocal(pg[:], sme[:]), deps=[cx])
                c = P.op(V, lambda e: e.tensor_scalar(oh4[:], dd[:], 0.0, None, ALU.is_equal), deps=[c])
                p4 = prod[:].rearrange("p (t g e) -> p t g e", t=T16, g=4)
                lf4 = lf.rearrange("p t (g e) -> p t g e", g=4)
                oh4b = oh4[:].rearrange("p (t g o) -> p t g o", t=T16, o=1).broadcast_to([128, T16, 4, 4])
                c = P.op(V, lambda e: e.tensor_tensor(p4, lf4, oh4b, ALU.mult), deps=[c])
                c = P.op(V, lambda e: e.tensor_reduce(v3(sel, 4), prod[:].rearrange("p (t g e) -> p t e g", t=T16, g=4), AX.X, ALU.add), deps=[c])
                c = P.op(V, lambda e: e.reduce_max(m1[:], v3(sel, 4), AX.X), deps=[c])
                c = P.op(V, lambda e: e.tensor_tensor(v3(d1, 4), v3(sel, 4), bc(m1, 4), ALU.subtract), deps=[c])
                c = P.op(V, lambda e: e.tensor_scalar(oh1[:], d1[:], 0.0, None, ALU.is_equal), deps=[c])
                c = P.op(V, lambda e: e.scalar_tensor_tensor(sel2[:], oh1[:], -1e30, sel[:], ALU.mult, ALU.add), deps=[c])
                c = P.op(V, lambda e: e.reduce_max(m2[:], v3(sel2, 4), AX.X), deps=[c])
                c = P.op(V, lambda e: e.tensor_tensor(v3(d2, 4), v3(sel2, 4), bc(m2, 4), ALU.subtract), deps=[c])
                c = P.op(V, lambda e: e.tensor_scalar(oh2[:], d2[:], 0.0, None, ALU.is_equal), deps=[c])
                c = P.op(V, lambda e: e.tensor_tensor(dm[:], m2[:], m1[:], ALU.subtract), deps=[c])
                c = P.op("scalar", lambda e: e.activation(ed[:], dm[:], AF.Exp), deps=[c])
                c = P.op(V, lambda e: e.tensor_scalar(w1[:], ed[:], 1.0, None, ALU.add), deps=[c])
                c = P.op(V, lambda e: e.reciprocal(w1[:], w1[:]), deps=[c])
                c = P.op(V, lambda e: e.tensor_tensor(w2[:], ed[:], w1[:], ALU.mult), deps=[c])
                c = P.op(V, lambda e: e.tensor_tensor(w1[:], w1[:], pg[:], ALU.mult), deps=[c, cx])
                c = P.op(V, lambda e: e.tensor_tensor(w2[:], w2[:], pg[:], ALU.mult), deps=[c])
                c = P.op(V, lambda e: e.tensor_tensor(v3(wi1, 4), v3(oh1, 4), bc(w1, 4), ALU.mult), deps=[c])
                c = P.op(V, lambda e: e.tensor_tensor(v3(wi2, 4), v3(oh2, 4), bc(w2, 4), ALU.mult), deps=[c])
                c = P.op(V, lambda e: e.tensor_tensor(wi1[:], wi1[:], wi2[:], ALU.add), deps=[c])
                g4 = gates[:].rearrange("p t (g e) -> p t g e", g=4)
                wib = wi1[:].rearrange("p (t o e) -> p t o e", t=T16, o=1).broadcast_to([128, T16, 4, 4])
                c = P.op(V, lambda e: e.tensor_tensor(g4, oh4b, wib, ALU.mult), deps=[c])
                c = P.op(V, lambda e: e.tensor_tensor(wi2[:], oh1[:], oh2[:], ALU.add), deps=[c])
                sm4 = selmask[:].rearrange("p t (g e) -> p t g e", g=4)
                w2b = wi2[:].rearrange("p (t o e) -> p t o e", t=T16, o=1).broadcast_to([128, T16, 4, 4])
                c = P.op(V, lambda e: e.tensor_tensor(sm4, oh4b, w2b, ALU.mult), deps=[c])

    def inproj_fm(wsl, dst, scale, evac_eng, bring, extra_deps, rotary, tbs=None):
        toks = []
        pending_rot = None

        def emit_rot(tb, te):
            rbank, rtmp, rt2, rstate = rotary
            rp = pbk[rbank][0:32, :]
            d32 = dst[0:32, tb * 512:(tb + 1) * 512]
            tp = P.op("tensor", lambda e: e.matmul(rp, PERM, d32, start=True, stop=True),
                      deps=[te, rstate["rel"]] + cdeps)
            ta = P.op("vector", lambda e: e.tensor_tensor(
                rtmp[:], rp, rot_tabs["sin"][:, tb * 512:(tb + 1) * 512], ALU.mult), deps=[tp, rstate["tmp_rel"], rot_tabs["tok"]])
            rstate["rel"] = [ta]
            tb2 = P.op("gpsimd", lambda e: e.tensor_tensor(
                rt2[:], d32, rot_tabs["cos"][:, tb * 512:(tb + 1) * 512], ALU.mult), deps=[te, rstate["tmp_rel"], rot_tabs["tok"]])
            tcomb = P.op("vector", lambda e: e.tensor_tensor(d32, rtmp[:], rt2[:], ALU.add), deps=[ta, tb2, tp])
            rstate["tmp_rel"] = [tcomb]
            return tcomb

        for tb in (range(4) if tbs is None else tbs):
            bi, bk, brel = bring.get()
            pb_ = pbk[bk]
            tm = None
            for kc in range(KC):
                tm = P.op("tensor", lambda e, pb_=pb_, kc=kc, tb=tb: e.matmul(
                    pb_, wsl[:, kc, :], xT[:, kc, tb * 512:(tb + 1) * 512], start=(kc == 0), stop=(kc == KC - 1)),
                    deps=[extra_deps, brel], sig=(kc == KC - 1))
            dv = dst[:, tb * 512:(tb + 1) * 512]
            if evac_eng == "scalar":
                if scale is None:
                    te = P.op("scalar", lambda e, dv=dv, pb_=pb_: e.copy(dv, pb_), deps=[tm, extra_deps])
                else:
                    te = P.op("scalar", lambda e, dv=dv, pb_=pb_: e.mul(dv, pb_, scale), deps=[tm, extra_deps])
            else:
                if scale is None:
                    te = P.op("vector", lambda e, dv=dv, pb_=pb_: e.tensor_copy(dv, pb_), deps=[tm, extra_deps])
                else:
                    te = P.op("vector", lambda e, dv=dv, pb_=pb_: e.tensor_scalar(dv, pb_, scale, None, ALU.mult), deps=[tm, extra_deps])
            bring.release(bi, [te])
            if rotary is not None:
                if pending_rot is not None:
                    toks.append(emit_rot(*pending_rot))
                pending_rot = (tb, te)
            else:
                toks.append(te)
        if rotary is not None and pending_rot is not None:
            toks.append(emit_rot(*pending_rot))
        return toks

    def tok_slice(dl, r, blk, n=1):
        start = r + dl * 128 * blk
        return slice(start, start + dl * (128 * n - 1) + 1, dl)

    def phase_mixer(l):
        oAT = P.sbuf("oAT", [128, 4, S], BF16)
        oBT = P.sbuf("oBT", [128, 8, S], BF16)
        for which in ("A", "B"):
            with P.phase():
                wsl = [P.sbuf(f"wsl{i}", [128, 3, KC, 128], BF16) for i in range(2)]
                wring = Ring(wsl)
                qk = [P.sbuf(f"qk{i}", [128, 2, S], BF16) for i in range(2)]
                qkring = Ring(qk)
                vv = [P.sbuf(f"vv{i}", [128, NT, 128], BF16) for i in range(2)]
                vring = Ring(vv)
                if which == "A":
                    ND = P.sbuf("ND", [128, 2, S], F32)
                    rot_tabs["cos"] = P.sbuf("cos_t", [32, S], F32)
                    rot_tabs["sin"] = P.sbuf("sin_t", [32, S], F32)
                    rot_tabs["tok"] = [P.dma("sync", rot_tabs["cos"][:], rot_d[0], stream="rotl"),
                                       P.dma("sync", rot_tabs["sin"][:], rot_d[1], stream="rotl")]
                    rtmp = P.sbuf("rtmp", [32, 512], F32)
                    rt2 = P.sbuf("rt2", [32, 512], F32)
                    pT = [P.sbuf(f"pT{i}", [128, 512], BF16) for i in range(3)]
                    pTring = Ring(pT)
                bring = Ring([0, 1, 2])
                rstate = {"rel": [], "tmp_rel": []}
                string = Ring([4, 5])
                pvring = Ring([6, 7])
                nd_rel = []

                if which == "A":
                    heads = [("A", g, j) for j in range(4) for g in range(3)]
                else:
                    heads = [("B", h, 0) for h in range(8)]

                def load_w(hd):
                    kind, a, b_ = hd
                    wi, wbuf, wrel = wring.get()
                    if kind == "A":
                        cq, ck, cv = a * 4 + b_, 12 + a * 4 + b_, 24 + a * 4 + b_
                    else:
                        cq, ck, cv = 36 + a, 44 + a, 52 + a
                    toks = []
                    for n, c in enumerate((cq, ck, cv)):
                        toks.append(P.dma("gpsimd", wbuf[:, n], win_d[l, c], deps=wrel, stream=f"wsl{wi}"))
                    return wi, wbuf, toks

                pending = load_w(heads[0])
                bstate = {}
                if which == "B":
                    ebuf = [P.sbuf(f"ebuf{i}", [128, 512], F32) for i in range(2)]
                    ering = Ring(ebuf)
                    spb = [P.sbuf(f"spb{i}", [128, 512], BF16) for i in range(3)]
                    cbuf = [P.sbuf(f"cbuf{i}", [128, 512], BF16) for i in range(3)]
                    abuf = [P.sbuf(f"abuf{i}", [128, 512], BF16) for i in range(3)]
                zring = Ring([3, 4, 5])
                oring = Ring([6, 7])

                for hi_, hd in enumerate(heads):
                    kind, a, b_ = hd
                    wi, wbuf, wtoks = pending
                    if hi_ + 1 < len(heads):
                        pending = load_w(heads[hi_ + 1])
                    if kind == "A":
                        qi, qkb, qrel = qkring.get()
                        vi, vb, vrel = vring.get()
                        qT = qkb[:, 0, :]
                        kT = qkb[:, 1, :]
                    if kind == "A":
                        g, j = a, b_
                        dl = DILS[g]
                        nb = 16 // dl
                        rot = (3, rtmp, rt2, rstate)
                        tq = inproj_fm(wbuf[:, 0], qT, SCALE, "scalar", bring, [wtoks, qrel], rot)
                        tk_ = inproj_fm(wbuf[:, 1], kT, None, "scalar", bring, [wtoks, qrel], rot)
                        tv = []
                        for t4 in range(4):
                            bi, bk, brel = bring.get()
                            pb_ = pbk[bk]
                            tm = None
                            for tq4 in range(4):
                                tau = t4 * 4 + tq4
                                r, blk = tau // nb, tau % nb
                                sl = tok_slice(dl, r, blk)
                                for kc in range(KC):
                                    tm = P.op("tensor", lambda e, pb_=pb_, tq4=tq4, kc=kc, sl=sl, wbuf=wbuf: e.matmul(
                                        pb_[:, tq4 * 128:(tq4 + 1) * 128], xT[:, kc, sl], wbuf[:, 2, kc, :],
                                        start=(kc == 0), stop=(kc == KC - 1)),
                                        deps=[wtoks, brel], sig=(tq4 == 3 and kc == KC - 1))
                            dv = vb[:, t4 * 4:(t4 + 1) * 4, :]
                            sv = pb_.rearrange("p (a b) -> p a b", a=4)
                            te = P.op("vector", lambda e, dv=dv, sv=sv: e.tensor_copy(dv, sv), deps=[tm, vrel])
                            bring.release(bi, [te])
                            tv.append(te)
                        wring.release(wi, [tm])
                        last_pe = None
                        tev = []

                        def emit_front(t2_, dl=dl, nb=nb, qT=qT, kT=kT, tq=tq, tk_=tk_):
                            taus = [2 * t2_, 2 * t2_ + 1]
                            si, sbk, srel = string.get()
                            sp_ = pbk[sbk]
                            col = 0
                            layout = []
                            specs = []
                            for tau in taus:
                                r, blk = tau // nb, tau % nb
                                parts = [(r, blk)] + ([(r, blk - 1)] if blk > 0 else [])
                                qsl = tok_slice(dl, r, blk)
                                pc = []
                                for (pr, pblk) in parts:
                                    specs.append((col, tok_slice(dl, pr, pblk), qsl))
                                    pc.append((col, pr * nb + pblk))
                                    col += 128
                                layout.append((tau, qsl, pc))
                            ts = None
                            for n_, (col_, ksl, qsl_) in enumerate(specs):
                                ts = P.op("tensor", lambda e, sp_=sp_, col_=col_, ksl=ksl, qsl_=qsl_, kT=kT, qT=qT: e.matmul(
                                    sp_[:, col_:col_ + 128], kT[:, ksl], qT[:, qsl_], start=True, stop=True),
                                    deps=[tq, tk_, srel], sig=(n_ == len(specs) - 1))
                            pi2, pTb, prel = pTring.get()
                            texp = P.op("scalar", lambda e, pTb=pTb, sp_=sp_, col=col: e.activation(
                                pTb[:, 0:col], sp_[:, 0:col], AF.Exp), deps=[ts, prel])
                            string.release(si, [texp])
                            tmk = []
                            for (tau, qsl, pc) in layout:
                                c0 = pc[0][0]
                                w_ = 128 * len(pc)
                                tmk.append(P.op("gpsimd", lambda e, pTb=pTb, c0=c0, w_=w_: e.tensor_tensor(
                                    pTb[:, c0:c0 + w_], pTb[:, c0:c0 + w_], M2[:, 0:w_], ALU.mult), deps=[texp] + cdeps))
                            return layout, pi2, pTb, tmk

                        def emit_back(ctx, g=g, vb=vb, tv=tv):
                            layout, pi2, pTb, tmk = ctx
                            vi2, vbk, vrel2 = pvring.get()
                            pv_ = pbk[vbk]
                            tpv = None
                            for n, (tau, qsl, pc) in enumerate(layout):
                                for wh in range(2):
                                    for m, (c_, ktau) in enumerate(pc):
                                        lhs = vb[:, ktau, :] if wh == 0 else ONES
                                        o_ = pv_[:, (n * 2 + wh) * 128:(n * 2 + wh + 1) * 128]
                                        tpv = P.op("tensor", lambda e, o_=o_, lhs=lhs, pTb=pTb, c_=c_, m=m, pc=pc: e.matmul(
                                            o_, lhs, pTb[:, c_:c_ + 128], start=(m == 0), stop=(m == len(pc) - 1)),
                                            deps=[tmk, tv, vrel2], sig=(n == 1 and wh == 1 and m == len(pc) - 1))
                            pTring.release(pi2, [tpv])
                            tev_l = []
                            for n, (tau, qsl, pc) in enumerate(layout):
                                src = pv_[:, n * 256:(n + 1) * 256].rearrange("p (a b) -> p a b", a=2)
                                dst = ND[:, :, qsl]
                                if g == 0:
                                    tev_l.append(P.op("vector", lambda e, dst=dst, src=src: e.tensor_copy(dst, src), deps=[tpv, nd_rel]))
                                else:
                                    tev_l.append(P.op("vector", lambda e, dst=dst, src=src: e.tensor_tensor(dst, src, dst, ALU.add), deps=[tpv]))
                            pvring.release(vi2, tev_l)
                            return tpv, tev_l

                        SK = 2
                        ctxs = {}
                        for step in range(8 + SK):
                            if step < 8:
                                ctxs[step] = emit_front(step)
                            if step - SK >= 0:
                                last_pe, tev = emit_back(ctxs.pop(step - SK))
                        qkring.release(qi, [last_pe])
                        vring.release(vi, [last_pe])
                        if l == 0 and hi_ == 0:
                            dbg_dump("qk", qkb[:])
                            dbg_dump("vv", vb[:])
                            dbg_dump("ND", ND[:])
                            if "qk" in dbg_d:
                                for _k in list(qkring.rel): pass
                                qkring.rel = [P.barrier()]
                                vring.rel = [P.barrier()]
                        if g == 2:
                            tf = P.op("vector", lambda e: e.reciprocal(ND[:, 1, :], ND[:, 1, :]), deps=[tev])
                            tf = P.op("vector", lambda e, j=j: e.tensor_tensor(oAT[:, j, :], ND[:, 0, :], ND[:, 1, :], ALU.mult), deps=[tf])
                            nd_rel = [tf]
                    else:
                        h = a
                        if False:
                            tail = P.barrier()
                            zring.rel = [list(tail) for _ in range(3)]
                            oring.rel = [list(tail) for _ in range(2)]
                        def b_prepare(pend):
                            wi_, wbuf_, wtoks_ = pend
                            qi_, qkb_, qrel_ = qkring.get()
                            vi_, vb_, vrel_ = vring.get()
                            stt = dict(wi=wi_, qi=qi_, vi=vi_, qkb=qkb_, vb=vb_, tq=[], tk=[], tv=[], lastv=None)
                            pieces = []
                            for tb in range(4):
                                pieces.append(lambda tb=tb: stt["tq"].extend(
                                    inproj_fm(wbuf_[:, 0], qkb_[:, 0, :], SCALE, "vector", bring, [wtoks_, qrel_], None, tbs=[tb])))
                            for tb in range(4):
                                pieces.append(lambda tb=tb: stt["tk"].extend(
                                    inproj_fm(wbuf_[:, 1], qkb_[:, 1, :], None, "vector", bring, [wtoks_, qrel_], None, tbs=[tb])))

                            def vpiece(t4):
                                bi, bk, brel = bring.get()
                                pb_ = pbk[bk]
                                tm = None
                                for tq4 in range(4):
                                    tau = t4 * 4 + tq4
                                    for kc in range(KC):
                                        tm = P.op("tensor", lambda e, pb_=pb_, tq4=tq4, kc=kc, tau=tau: e.matmul(
                                            pb_[:, tq4 * 128:(tq4 + 1) * 128], xT[:, kc, tau * 128:(tau + 1) * 128], wbuf_[:, 2, kc, :],
                                            start=(kc == 0), stop=(kc == KC - 1)),
                                            deps=[wtoks_, brel], sig=(tq4 == 3 and kc == KC - 1))
                                dv = vb_[:, t4 * 4:(t4 + 1) * 4, :]
                                sv = pb_.rearrange("p (a b) -> p a b", a=4)
                                te = P.op("vector", lambda e, dv=dv, sv=sv: e.tensor_copy(dv, sv), deps=[tm, vrel_])
                                bring.release(bi, [te])
                                stt["tv"].append(te)
                                stt["lastv"] = tm
                            for t4 in range(4):
                                pieces.append(lambda t4=t4: vpiece(t4))
                            stt["pieces"] = pieces
                            return stt

                        def run_pieces(stt, n=None):
                            k_ = 0
                            while stt["pieces"] and (n is None or k_ < n):
                                stt["pieces"].pop(0)()
                                k_ += 1
                            if not stt["pieces"] and not stt.get("released"):
                                wring.release(stt["wi"], [stt["lastv"]])
                                stt["released"] = True

                        if bstate.get("cur") is None:
                            cur = b_prepare((wi, wbuf, wtoks))
                            run_pieces(cur)
                        else:
                            cur = bstate["cur"]
                            run_pieces(cur)
                        nxt = b_prepare(pending) if hi_ + 1 < len(heads) else None
                        bstate["cur"] = nxt
                        qi, vi, qkb, vb = cur["qi"], cur["vi"], cur["qkb"], cur["vb"]
                        qT = qkb[:, 0, :]
                        kT = qkb[:, 1, :]
                        tq, tk_, tv = cur["tq"], cur["tk"], cur["tv"]
                        items = []
                        for qc in range(4):
                            kbs = list(range(4 * qc + 3, -1, -1))
                            for idx, kb in enumerate(kbs):
                                c0 = max(128 * kb - 512 * qc, 0)
                                items.append(dict(qc=qc, kb=kb, idx=idx, c0=c0, diag=(kb >= 4 * qc), last=(idx == len(kbs) - 1)))
                        n_it = len(items)
                        st = {"crel": {}, "sprel": {}, "arel": {}, "obank": None}
                        last_tokens = []

                        def stageA(s):
                            it = items[s]
                            zi, zbk, zrel = zring.get()
                            it["zi"], it["z"] = zi, pbk[zbk]
                            c0, qc, kb = it["c0"], it["qc"], it["kb"]
                            z = it["z"]
                            it["tA"] = P.op("tensor", lambda e, kT=kT, qT=qT: e.matmul(
                                z[:, c0:512], kT[:, kb * 128:(kb + 1) * 128], qT[:, qc * 512 + c0:(qc + 1) * 512],
                                start=True, stop=True), deps=[tq, tk_, zrel])

                        def stageB(s):
                            it = items[s]
                            c0, z = it["c0"], it["z"]
                            ei, eb, erel = ering.get()
                            spx = spb[s % 3]
                            it["sp"] = spx
                            t1_ = P.op("scalar", lambda e: e.activation(eb[:, c0:512], z[:, c0:512], AF.Exp), deps=[it["tA"], erel])
                            t2_ = P.op("scalar", lambda e: e.activation(spx[:, c0:512], eb[:, c0:512], AF.Ln, bias=1.0),
                                       deps=[t1_, st["sprel"].get(s % 3)])
                            ering.release(ei, [t2_])
                            if it["diag"]:
                                t2_ = P.op("gpsimd", lambda e: e.tensor_tensor(
                                    spx[:, c0:c0 + 128], spx[:, c0:c0 + 128], MSTRICT, ALU.mult), deps=[t2_] + cdeps)
                            it["tB"] = t2_
                            cn = cbuf[s % 3]
                            it["C"] = cn
                            crel = st["crel"].get(s % 3)
                            if it["idx"] == 0:
                                it["tC9"] = P.op("vector", lambda e: e.tensor_copy(cn[:, c0:512], spx[:, c0:512]), deps=[t2_, crel])
                            else:
                                pit = items[s - 1]
                                c1 = pit["c0"]
                                cp = pit["C"]
                                tt_ = P.op("vector", lambda e: e.tensor_tensor(cn[:, c1:512], cp[:, c1:512], spx[:, c1:512], ALU.add),
                                           deps=[t2_, pit["tC9"], crel])
                                if c0 < c1:
                                    tt_ = P.op("vector", lambda e: e.tensor_copy(cn[:, c0:c1], spx[:, c0:c1]), deps=[t2_, crel, tt_])
                                it["tC9"] = tt_

                        def stageC(s):
                            it = items[s]
                            c0, z, spx = it["c0"], it["z"], it["sp"]
                            tc = P.op("tensor", lambda e: e.matmul(z[:, c0:512], NEGU, spx[:, c0:512], start=False, stop=True,
                                                                   skip_group_check=True), deps=[it["tB"]] + cdeps, sig=(it["idx"] == 0))
                            if it["idx"] > 0:
                                pit = items[s - 1]
                                c1, cp = pit["c0"], pit["C"]
                                tc = P.op("tensor", lambda e: e.matmul(z[:, c1:512], NEGONES, cp[:, c1:512], start=False, stop=True,
                                                                       skip_group_check=True), deps=[pit["tC9"]])
                            it["tC"] = tc
                            st["sprel"][s % 3] = [tc, it["tC9"]]
                            if s >= 1:
                                st["crel"][(s - 1) % 3] = [tc]

                        def stageD(s):
                            it = items[s]
                            c0, z = it["c0"], it["z"]
                            ab = abuf[s % 3]
                            it["A"] = ab
                            td = P.op("scalar", lambda e: e.activation(ab[:, c0:512], z[:, c0:512], AF.Exp),
                                      deps=[it["tC"], st["arel"].get(s % 3)])
                            zring.release(it["zi"], [td])
                            if it["diag"]:
                                td = P.op("gpsimd", lambda e: e.tensor_tensor(
                                    ab[:, c0:c0 + 128], ab[:, c0:c0 + 128], MSTRICT, ALU.mult), deps=[td] + cdeps)
                            it["tD"] = td

                        def stageE(s):
                            it = items[s]
                            c0, ab, kb, qc = it["c0"], it["A"], it["kb"], it["qc"]
                            if it["idx"] == 0:
                                oi, obk, orel = oring.get()
                                st["obank"] = (oi, pbk[obk], orel)
                            oi, ob, orel = st["obank"]
                            te_ = P.op("tensor", lambda e, vb=vb: e.matmul(ob[:, c0:512], vb[:, kb, :], ab[:, c0:512],
                                                                    start=(it["idx"] == 0), stop=it["last"], skip_group_check=True),
                                       deps=[it["tD"], tv, orel])
                            st["arel"][s % 3] = [te_]
                            if it["last"]:
                                tev_ = P.op("vector", lambda e, h=h: e.tensor_copy(oBT[:, h, qc * 512:(qc + 1) * 512], ob), deps=[te_])
                                oring.release(oi, [tev_])
                                last_tokens.append(te_)

                        for s in range(n_it + 2):
                            if s < n_it:
                                stageA(s)
                                stageB(s)
                            if 0 <= s - 1 < n_it:
                                stageC(s - 1)
                                stageD(s - 1)
                            if 0 <= s - 2 < n_it:
                                stageE(s - 2)
                            if nxt is not None and s % 3 == 2:
                                run_pieces(nxt, 1)
                        if nxt is not None:
                            run_pieces(nxt)
                        qkring.release(qi, [last_tokens[-1]])
                        vring.release(vi, [last_tokens[-1]])
                if l == 0:
                    dbg_dump("oAT", oAT[:], None)
                    dbg_dump("oBT", oBT[:], None)
        if stop_after == "attn":
            return

        with P.phase():
            wg = [P.sbuf(f"wg{i}", [128, 2, KC, 128], BF16) for i in range(2)]
            wgring = Ring(wg)
            wpa = [P.sbuf(f"wpa{i}", [128, 4, 128], BF16) for i in range(2)]
            wpb = [P.sbuf(f"wpb{i}", [128, 8, 128], BF16) for i in range(2)]
            sg = [P.sbuf(f"sg{i}", [128, 2, 512], F32) for i in range(2)]
            sgring = Ring(sg)
            mm = [P.sbuf(f"mm{i}", [128, 2, 512], F32) for i in range(2)]
            mmring = Ring(mm)
            mo = [P.sbuf(f"mo{i}", [128, 512], BF16) for i in range(2)]
            moring = Ring(mo)
            bset = Ring([(0, 1, 2, 3), (4, 5, 6, 7)])

            def load_c(c):
                wi, wb, wrel = wgring.get()
                toks = [P.dma("gpsimd", wb[:, 0], win_d[l, 60 + c], deps=wrel, stream=f"wg{wi}"),
                        P.dma("gpsimd", wb[:, 1], win_d[l, 76 + c], deps=wrel, stream=f"wg{wi}"),
                        P.dma("gpsimd", wpa[wi][:], pa_d[l, c], deps=wrel, stream=f"wg{wi}"),
                        P.dma("gpsimd", wpb[wi][:], pb_d[l, c], deps=wrel, stream=f"wg{wi}")]
                return wi, wb, toks

            pend = load_c(0)
            for c in range(16):
                wi, wb, wtoks = pend
                if c + 1 < 16:
                    pend = load_c(c + 1)
                tlast = None
                for tb in range(4):
                    bi, banks, brel = bset.get()
                    tsl = slice(tb * 512, (tb + 1) * 512)
                    tms = []
                    for n in range(2):
                        pb_ = pbk[banks[n]]
                        for kc in range(KC):
                            tm = P.op("tensor", lambda e, pb_=pb_, n=n, kc=kc, tsl=tsl, wb=wb: e.matmul(
                                pb_, wb[:, n, kc, :], xT[:, kc, tsl], start=(kc == 0), stop=(kc == KC - 1)),
                                deps=[wtoks, brel], sig=(kc == KC - 1))
                        tms.append(tm)
                    pb_ = pbk[banks[2]]
                    for kc in range(4):
                        tm = P.op("tensor", lambda e, pb_=pb_, kc=kc, tsl=tsl, wi=wi: e.matmul(
                            pb_, wpa[wi][:, kc, :], oAT[:, kc, tsl], start=(kc == 0), stop=(kc == 3)),
                            deps=[wtoks, brel], sig=(kc == 3))
                    tms.append(tm)
                    pb_ = pbk[banks[3]]
                    for kc in range(8):
                        tm = P.op("tensor", lambda e, pb_=pb_, kc=kc, tsl=tsl, wi=wi: e.matmul(
                            pb_, wpb[wi][:, kc, :], oBT[:, kc, tsl], start=(kc == 0), stop=(kc == 7)),
                            deps=[wtoks, brel], sig=(kc == 7))
                    tms.append(tm)
                    tlast = tm
                    si, sgb, srel = sgring.get()
                    ta = P.op("scalar", lambda e, sgb=sgb, b0=pbk[banks[0]]: e.activation(sgb[:, 0, :], b0, AF.Sigmoid), deps=[tms[0], srel])
                    tb_ = P.op("scalar", lambda e, sgb=sgb, b1=pbk[banks[1]]: e.activation(sgb[:, 1, :], b1, AF.Sigmoid), deps=[tms[1], srel])
                    mi, mmb, mrel = mmring.get()
                    tc = P.op("vector", lambda e, mmb=mmb, sgb=sgb, b2=pbk[banks[2]]: e.tensor_tensor(mmb[:, 0, :], sgb[:, 0, :], b2, ALU.mult), deps=[ta, tms[2], mrel])
                    td = P.op("vector", lambda e, mmb=mmb, sgb=sgb, b3=pbk[banks[3]]: e.tensor_tensor(mmb[:, 1, :], sgb[:, 1, :], b3, ALU.mult), deps=[tb_, tms[3], mrel])
                    bset.release(bi, [ta, tb_, tc, td])
                    sgring.release(si, [tc, td])
                    oi, mob, orel = moring.get()
                    te = P.op("gpsimd", lambda e, mob=mob, mmb=mmb: e.tensor_tensor(mob[:], mmb[:, 0, :], mmb[:, 1, :], ALU.add), deps=[tc, td, orel])
                    mmring.release(mi, [te])
                    tdm = P.dma("sync", mT_d[tb * 4:(tb + 1) * 4, :, c, :].rearrange("t p n -> p t n"),
                                mob[:].rearrange("p (t n) -> p t n", t=4), deps=[te], stream=f"mo{oi}")
                    moring.release(oi, [tdm])
                wgring.release(wi, [tlast])
            if l == 0:
                dbg_dump("mT", mT_d)

    def phase_mixer_out(l, res_src_d, res_dst_d):
        with P.phase():
            wo = P.sbuf("wo", [128, KC, D], BF16)
            two = [P.dma("gpsimd", wo[:, :, nb4 * 512:(nb4 + 1) * 512], wo_d[l, :, :, nb4 * 512:(nb4 + 1) * 512], stream=f"wo{nb4}") for nb4 in range(4)]
            mt = [P.sbuf(f"mt{i}", [128, KC, 128], BF16) for i in range(2)]
            mtring = Ring(mt)
            yring = Ring([0, 1])
            ln = LNEpilogue(l, 0, res_src_d, res_dst_d)
            mtpf = {}

            def mt_prefetch(tt):
                mi, mtb, mrel = mtring.get()
                tl = P.dma("sync", mtb[:], mT_d[tt], deps=mrel, stream=f"mt{mi}")
                mtpf[tt] = (mi, mtb, tl)

            mt_prefetch(0)
            ln.prefetch(0)
            for tt in range(NT):
                if tt + 1 < NT:
                    mt_prefetch(tt + 1)
                    ln.prefetch(tt + 1)
                mi, mtb, tl = mtpf.pop(tt)
                yi, yh, yrel = yring.get()
                yp = pall[:, yh * 2048:(yh + 1) * 2048]
                tm = None
                for nb_ in range(4):
                    for kc in range(KC):
                        tm = P.op("tensor", lambda e, yp=yp, nb_=nb_, kc=kc, mtb=mtb: e.matmul(
                            yp[:, nb_ * 512:(nb_ + 1) * 512], mtb[:, kc, :], wo[:, kc, nb_ * 512:(nb_ + 1) * 512],
                            start=(kc == 0), stop=(kc == KC - 1)), deps=[tl, two[nb_], yrel], sig=(nb_ == 3 and kc == KC - 1))
                mtring.release(mi, [tm])
                tz = ln.run(tt, yp, [tm])
                yring.release(yi, [tz])
            ln.done()

    class LNEpilogue:
        def __init__(self, l, which, res_src_d, res_dst_d, store_q="scalar"):
            self.res_src_d, self.res_dst_d = res_src_d, res_dst_d
            self.store_q = store_q
            self.gb = P.sbuf("ln_gbuf", [128, 2, D], F32)
            self.tg = [P.dma("sync", self.gb[:, 0, :], lng_d[l, which, 0], stream="lng"),
                       P.dma("sync", self.gb[:, 1, :], lng_d[l, which, 1], stream="lng")]
            self.hr = [P.sbuf(f"hr{i}", [128, D], F32) for i in range(2)]
            self.hring = Ring(self.hr)
            self.zb = [P.sbuf(f"zb{i}", [128, D], F32) for i in range(3)]
            self.zring = Ring(self.zb)
            self.stt = [P.sbuf(f"lnst{i}", [128, 40], F32) for i in range(3)]
            self.sring = Ring(self.stt)
            self.toks = []
            self.pf = {}

        def prefetch(self, tt):
            hi, hb, hrel = self.hring.get()
            tl = P.dma("sync", hb[:], self.res_src_d[tt * 128:(tt + 1) * 128, :], deps=hrel, stream=f"hr{hi}")
            self.pf[tt] = (hi, hb, tl)

        def run(self, tt, y_ap, ydeps, y_is_psum=True):
            if tt not in self.pf:
                self.prefetch(tt)
            hi, hb, tl = self.pf.pop(tt)
            zi, zb, zrel = self.zring.get()
            si, stt, srel = self.sring.get()
            tz = P.op("vector", lambda e: e.scalar_tensor_tensor(zb[:], hb[:], ALPHA, y_ap, ALU.mult, ALU.add), deps=[tl, ydeps, zrel])
            self.hring.release(hi, [tz])
            ts = None
            for c in range(4):
                ts = P.op("vector", lambda e, c=c: e.bn_stats(stt[:, c * 6:(c + 1) * 6], zb[:, c * 512:(c + 1) * 512]), deps=[tz, srel])
            mv = stt[:, 24:26]
            ts = P.op("vector", lambda e: e.bn_aggr(mv, stt[:, 0:24]), deps=[ts])
            sd = stt[:, 26:27]
            rstd = stt[:, 27:28]
            nmr = stt[:, 28:29]
            ta = P.op("scalar", lambda e: e.activation(sd, stt[:, 25:26], AF.Sqrt, bias=LN_EPS, scale=1.0), deps=[ts])
            tb_ = P.op("vector", lambda e: e.reciprocal(rstd, sd), deps=[ta])
            tb_ = P.op("vector", lambda e: e.scalar_tensor_tensor(nmr, stt[:, 24:25], -1.0, rstd, ALU.mult, ALU.mult), deps=[tb_])
            tn = P.op("scalar", lambda e: e.activation(zb[:], zb[:], AF.Identity, bias=nmr, scale=rstd), deps=[tb_])
            tg_ = P.op("vector", lambda e: e.tensor_tensor(zb[:], zb[:], self.gb[:, 0, :], ALU.mult), deps=[tn, self.tg])
            tb2 = P.op("vector", lambda e: e.tensor_tensor(zb[:], zb[:], self.gb[:, 1, :], ALU.add), deps=[tg_])
            self.sring.release(si, [tb2])
            td = P.dma(self.store_q, self.res_dst_d[tt * 128:(tt + 1) * 128, :], zb[:], deps=[tb2], stream=f"zb{zi}")
            self.zring.release(zi, [td])
            self.toks.append(td)
            return tz

        def done(self):
            pass

    def phase_moe(l, res_src_d, res_dst_d, final):
        yacc = P.sbuf("yacc", [128, 8, D], F32)
        ln_toks = []
        for half in range(2):
            with P.phase():
                wu = [P.sbuf(f"wu{i}", [128, KC, 2, 128], BF16) for i in range(2)]
                wuring = Ring(wu)
                wd = [P.sbuf(f"wd{i}", [128, 4, 1024], BF16) for i in range(2)]
                wdring = Ring(wd)
                act = P.sbuf("act", [128, 4, 1024], BF16)
                sl_ = [P.sbuf(f"silu{i}", [128, 512], F32) for i in range(2)]
                slring = Ring(sl_)
                upring = Ring([(0, 1), (2, 3)])
                dnring = Ring([2, 3])
                act_rel = []
                jobs = []
                for e_ in range(16):
                    for i in range(4):
                        jobs.append(("u", e_, i))
                    for d_ in range(2):
                        jobs.append(("d", e_, d_))
                loaded = {}

                def issue(job):
                    kind, e_, k = job
                    if kind == "u":
                        wi, wb, wrel = wuring.get()
                        t = P.dma("gpsimd", wb[:], wup_d[l, e_, k], deps=wrel, stream=f"wu{wi}")
                        loaded[job] = (wuring, wi, wb, [t])
                    else:
                        wi, wb, wrel = wdring.get()
                        t = P.dma("gpsimd", wb[:], wdn_d[l, e_, :, k * 1024:(k + 1) * 1024].rearrange("(c p) n -> p c n", p=128),
                                  deps=wrel, stream=f"wd{wi}")
                        loaded[job] = (wdring, wi, wb, [t])

                PF = 2
                nu = 0
                for jn in range(min(PF, len(jobs))):
                    issue(jobs[jn])
                nxt = min(PF, len(jobs))
                act_w = []
                dn_last = []
                for jn, job in enumerate(jobs):
                    kind, e_, k = job
                    ring_, wi, wb, wtoks = loaded.pop(job)
                    if kind == "u":
                        i = k
                        tlast = None
                        for tbh in range(2):
                            tsl = slice(half * 1024 + tbh * 512, half * 1024 + (tbh + 1) * 512)
                            ui, banks, urel = upring.get()
                            tms = []
                            for n in range(2):
                                pb_ = pbk[banks[n]]
                                for kc in range(KC):
                                    tm = P.op("tensor", lambda e, pb_=pb_, n=n, kc=kc, tsl=tsl, wb=wb: e.matmul(
                                        pb_, wb[:, kc, n, :], xT[:, kc, tsl], start=(kc == 0), stop=(kc == KC - 1)),
                                        deps=[wtoks, urel], sig=(kc == KC - 1))
                                tms.append(tm)
                            tlast = tm
                            si, sb_, srel = slring.get()
                            ta = P.op("scalar", lambda e, sb_=sb_, b0=pbk[banks[0]]: e.activation(sb_[:], b0, AF.Silu), deps=[tms[0], srel])
                            av = act[:, i, tbh * 512:(tbh + 1) * 512]
                            tb_ = P.op("vector", lambda e, av=av, sb_=sb_, b1=pbk[banks[1]]: e.tensor_tensor(av, sb_[:], b1, ALU.mult),
                                       deps=[ta, tms[1], dn_last])
                            slring.release(si, [tb_])
                            upring.release(ui, [ta, tb_])
                            act_w.append(tb_)
                        ring_.release(wi, [tlast])
                    else:
                        d_ = k
                        tlast = None
                        tevs = []
                        for t8 in range(8):
                            di, dh, drel = dnring.get()
                            yp = pall[:, dh * 1024:(dh + 1) * 1024]
                            tm = None
                            for nb_ in range(2):
                                for c in range(4):
                                    tm = P.op("tensor", lambda e, yp=yp, nb_=nb_, c=c, t8=t8, wb=wb: e.matmul(
                                        yp[:, nb_ * 512:(nb_ + 1) * 512], act[:, c, t8 * 128:(t8 + 1) * 128],
                                        wb[:, c, nb_ * 512:(nb_ + 1) * 512], start=(c == 0), stop=(c == 3)),
                                        deps=[wtoks, act_w, drel], sig=(nb_ == 1 and c == 3))
                            tlast = tm
                            ya = yacc[:, t8, d_ * 1024:(d_ + 1) * 1024]
                            gsc = gates[:, half * 8 + t8, e_:e_ + 1]
                            if e_ == 0:
                                tev = P.op("vector", lambda e, ya=ya, yp=yp, gsc=gsc: e.tensor_scalar(ya, yp, gsc, None, ALU.mult), deps=[tm, ln_toks])
                            else:
                                tev = P.op("vector", lambda e, ya=ya, yp=yp, gsc=gsc: e.scalar_tensor_tensor(ya, yp, gsc, ya, ALU.mult, ALU.add), deps=[tm])
                            dnring.release(di, [tev])
                            tevs.append(tev)
                        ring_.release(wi, [tlast])
                        if d_ == 1:
                            dn_last = [tlast]
                            act_w = []
                    if nxt < len(jobs):
                        issue(jobs[nxt])
                        nxt += 1
            with P.phase():
                ln = LNEpilogue(l, 1, res_src_d, res_dst_d)
                ln.prefetch(half * 8)
                for t8 in range(8):
                    if t8 + 1 < 8:
                        ln.prefetch(half * 8 + t8 + 1)
                    tz = ln.run(half * 8 + t8, yacc[:, t8, :], [])
                    ln_toks.append(tz)
                if final:
                    out_toks.extend(ln.toks)


    def phase_moe_sparse(l, res_src_d, res_dst_d, final):
        V = "vector"
        IOA = bass.IndirectOffsetOnAxis
        with P.phase():
            SHI = P.sbuf("SHI", [128, NT], I32)
            SLO = P.sbuf("SLO", [128, NT], I32)
            GHI = P.sbuf("GHI", [128, NT], F32)
            GLO = P.sbuf("GLO", [128, NT], F32)
            IDXU = P.sbuf("IDXU", [128, KT * 8], I32)
            IDXD = P.sbuf("IDXD", [128, KT * 4], I32)
            with P.phase():
                def W(name, n, dt=F32):
                    return P.sbuf("r_" + name, [128, n], dt)

                def v3(t, a):
                    return t[:].rearrange("p (a b) -> p a b", a=a)

                gfl = gates[:].rearrange("p t e -> p (t e)")
                Mf = selmask[:].rearrange("p t e -> p (t e)")
                Mb = W("Mb", NT * 16, BF16)
                c0 = P.op(V, lambda e: e.tensor_copy(Mb[:], Mf))
                rk = pbk[0]
                cn = pbk[1]
                tm = None
                for tt in range(NT):
                    tm = P.op("tensor", lambda e, tt=tt: e.matmul(rk[:, tt * 16:(tt + 1) * 16], MSTRICT, Mb[:, tt * 16:(tt + 1) * 16],
                                                                start=True, stop=(tt == 0)), deps=[c0] + cdeps, sig=False)
                    for t2 in range(tt):
                        tm = P.op("tensor", lambda e, tt=tt, t2=t2: e.matmul(rk[:, tt * 16:(tt + 1) * 16], ONES, Mb[:, t2 * 16:(t2 + 1) * 16],
                                                                         start=False, stop=(t2 == tt - 1)), sig=False)
                for tt in range(NT):
                    tm = P.op("tensor", lambda e, tt=tt: e.matmul(cn[:, 0:16], ONES, Mb[:, tt * 16:(tt + 1) * 16],
                                                                start=(tt == 0), stop=(tt == NT - 1)), sig=(tt == NT - 1))
                nf, tmpf, ptf, ts, ends, base1 = W("nf", 16), W("tmpf", 16), W("ptf", 16), W("ts", 16), W("ends", 16), W("base1", 16)
                pti = W("pti", 16, I32)
                c = P.op(V, lambda e: e.tensor_scalar(tmpf[:], cn[:, 0:16], 255.0, None, ALU.add), deps=[tm])
                c = P.op(V, lambda e: e.tensor_copy(pti[:], tmpf[:]), deps=[c])
                c = P.op(V, lambda e: e.tensor_scalar(pti[:], pti[:], 8, None, ALU.arith_shift_right), deps=[c])
                c = P.op(V, lambda e: e.tensor_copy(ptf[:], pti[:]), deps=[c])
                c = P.op(V, lambda e: e.tensor_scalar(ts[:, 0:1], ptf[:, 0:1], 0.0, None, ALU.mult), deps=[c])
                for e_ in range(1, 16):
                    c = P.op(V, lambda e, e_=e_: e.tensor_tensor(ts[:, e_:e_ + 1], ts[:, e_ - 1:e_], ptf[:, e_ - 1:e_], ALU.add), deps=[c])
                c = P.op(V, lambda e: e.tensor_tensor(ends[:], ts[:], ptf[:], ALU.add), deps=[c])
                c = P.op(V, lambda e: e.tensor_scalar(base1[:], ts[:], float(TS), 1.0, ALU.mult, ALU.add), deps=[c])
                SL, A_, eq = W("SL", 256), W("A", 256), W("eq", 256)
                amax, asum, tf1, tf2, gsum = W("amax", 16), W("asum", 16), W("tf1", 16), W("tf2", 16), W("gsum", 16)
                b1b = base1[:].rearrange("p (o e) -> p o e", o=1).broadcast_to([128, NT, 16])
                c = P.op(V, lambda e: e.tensor_tensor(v3(SL, NT), rk[:, 0:256].rearrange("p (a b) -> p a b", a=NT), b1b, ALU.add), deps=[c])
                c = P.op(V, lambda e: e.tensor_tensor(A_[:], SL[:], Mf, ALU.mult), deps=[c])
                c = P.op(V, lambda e: e.reduce_max(amax[:], v3(A_, NT), AX.X), deps=[c])
                c = P.op(V, lambda e: e.reduce_sum(asum[:], v3(A_, NT), AX.X), deps=[c])
                c = P.op(V, lambda e: e.tensor_scalar(tf1[:], amax[:], -1.0, None, ALU.add), deps=[c])
                c = P.op(V, lambda e: e.tensor_copy(SHI[:], tf1[:]), deps=[c])
                c = P.op(V, lambda e: e.tensor_tensor(tf2[:], asum[:], amax[:], ALU.subtract), deps=[c])
                c = P.op(V, lambda e: e.tensor_scalar(tf2[:], tf2[:], -1.0, None, ALU.add), deps=[c])
                c = P.op(V, lambda e: e.tensor_copy(SLO[:], tf2[:]), deps=[c])
                amb = amax[:].rearrange("p (t o) -> p t o", o=1).broadcast_to([128, NT, 16])
                c = P.op(V, lambda e: e.tensor_tensor(v3(eq, NT), v3(A_, NT), amb, ALU.is_equal), deps=[c])
                c = P.op(V, lambda e: e.tensor_tensor(eq[:], eq[:], gfl, ALU.mult), deps=[c])
                c = P.op(V, lambda e: e.reduce_sum(GHI[:], v3(eq, NT), AX.X), deps=[c])
                c = P.op(V, lambda e: e.reduce_sum(gsum[:], gates[:], AX.X), deps=[c])
                c = P.op(V, lambda e: e.tensor_tensor(GLO[:], gsum[:], GHI[:], ALU.subtract), deps=[c])
                cmp_, ek, eku = W("cmp", KT * 16), W("ek", KT), W("eku", KT)
                enb = ends[:].rearrange("p (o e) -> p o e", o=1).broadcast_to([128, KT, 16])
                kgb = cscal[:, 8:8 + KT].rearrange("p (k o) -> p k o", o=1).broadcast_to([128, KT, 16])
                c = P.op(V, lambda e: e.tensor_tensor(v3(cmp_, KT), enb, kgb, ALU.is_le), deps=[c] + cdeps)
                c = P.op(V, lambda e: e.reduce_sum(ek[:], v3(cmp_, KT), AX.X), deps=[c])
                un = W("un", KT)
                c = P.op(V, lambda e: e.tensor_scalar(un[:], ek[:], 15.5, 1.0e6, ALU.is_gt, ALU.mult), deps=[c])
                c = P.op(V, lambda e: e.tensor_scalar(ek[:], ek[:], 15.0, None, ALU.min), deps=[c])
                iuf, idf = W("iuf", KT * 8), W("idf", KT * 4)
                c = P.op(V, lambda e: e.tensor_scalar(eku[:], ek[:], 1024.0, float(l * 16384), ALU.mult, ALU.add), deps=[c])
                c = P.op(V, lambda e: e.tensor_tensor(eku[:], eku[:], un[:], ALU.add), deps=[c])
                ekb8 = eku[:].rearrange("p (k o) -> p k o", o=1).broadcast_to([128, KT, 8])
                pj8 = cscal[:, 40:48].rearrange("p (o j) -> p o j", o=1).broadcast_to([128, KT, 8])
                c = P.op(V, lambda e: e.tensor_tensor(v3(iuf, KT), ekb8, pj8, ALU.add), deps=[c])
                c = P.op(V, lambda e: e.tensor_copy(IDXU[:], iuf[:]), deps=[c])
                c = P.op(V, lambda e: e.tensor_scalar(eku[:], ek[:], 512.0, float(l * 8192), ALU.mult, ALU.add), deps=[c])
                c = P.op(V, lambda e: e.tensor_tensor(eku[:], eku[:], un[:], ALU.add), deps=[c])
                ekb4 = eku[:].rearrange("p (k o) -> p k o", o=1).broadcast_to([128, KT, 4])
                pj4 = cscal[:, 40:44].rearrange("p (o j) -> p o j", o=1).broadcast_to([128, KT, 4])
                c = P.op(V, lambda e: e.tensor_tensor(v3(idf, KT), ekb4, pj4, ALU.add), deps=[c])
                c = P.op(V, lambda e: e.tensor_copy(IDXD[:], idf[:]), deps=[c])
            if l == 0:
                dbg_dump("SHI", SHI[:])
                dbg_dump("SLO", SLO[:])
                dbg_dump("IDXU", IDXU[:])
            with P.phase():
                sxb = [P.sbuf(f"sx{i}", [128, D], F32) for i in range(2)]
                sring = Ring(sxb)
                sx16 = [P.sbuf(f"sxh{i}", [128, D], BF16) for i in range(2)]
                s16ring = Ring(sx16)
                for tt in range(NT):
                    i, buf, rel = sring.get()
                    tl = P.dma("sync", buf[:], res_src_d[tt * 128:(tt + 1) * 128, :], deps=rel, stream=f"sx{i}")
                    i2, b16, rel2 = s16ring.get()
                    if tt % 2 == 0:
                        tc_ = P.op("scalar", lambda e, b16=b16, buf=buf: e.copy(b16[:], buf[:]), deps=[tl, rel2])
                    else:
                        tc_ = P.op("vector", lambda e, b16=b16, buf=buf: e.tensor_copy(b16[:], buf[:]), deps=[tl, rel2])
                    sring.release(i, [tc_])
                    s1 = P.dma_fn("gpsimd", lambda e, b16=b16, tt=tt: e.indirect_dma_start(
                        out=XS_d[:, :], out_offset=IOA(ap=SHI[:, tt:tt + 1], axis=0), in_=b16[:, :], in_offset=None), deps=[tc_], stream=f"sc{i2}")
                    s2 = P.dma_fn("gpsimd", lambda e, b16=b16, tt=tt: e.indirect_dma_start(
                        out=XS_d[:, :], out_offset=IOA(ap=SLO[:, tt:tt + 1], axis=0), in_=b16[:, :], in_offset=None), deps=[tc_], stream=f"sc{i2}")
                    s16ring.release(i2, [s1, s2])
            with P.phase():
                wu3 = P.sbuf("swu3", [128, 8, 2048], BF16)
                wu = [xT[:, 0:8, :], xT[:, 8:16, :], wu3[:]]
                wd = [P.sbuf(f"swd{i}", [128, 4, 2048], BF16) for i in range(3)]
                wring = Ring([0, 1, 2])
                xtok = [P.sbuf(f"sxt{i}", [128, 2, D], BF16) for i in range(2)]
                xring = Ring(xtok)
                xsT = [P.sbuf(f"sxT{i}", [128, KC, TS], BF16) for i in range(2)]
                xTring = Ring(xsT)
                act = [P.sbuf(f"sact{i}", [128, 4, TS], BF16) for i in range(2)]
                aring = Ring(act)
                sl_ = [P.sbuf(f"ssl{i}", [128, TS], F32) for i in range(2)]
                slring = Ring(sl_)
                yst = [P.sbuf(f"syst{i}", [128, 1024], F32) for i in range(3)]
                yring = Ring(yst)
                upring = Ring([(0, 1), (2, 3)])
                dnring = Ring([2, 3])
                IDB = cbf[:, 896:1024]

                breg = {}

                def get_breg(e):
                    if "r" not in breg:
                        breg["r"] = e.to_reg(L * 16384 - 1)
                    return breg["r"]

                def gather_b(e, out_ap, in_ap, idx_ap):
                    reg = e.to_reg(L * 16384 - 1)
                    ins = e.indirect_dma_start(out=out_ap, out_offset=None, in_=in_ap,
                                               in_offset=IOA(ap=idx_ap, axis=0), bounds_check=reg, oob_is_err=False)
                    e.free_register(reg)
                    return ins

                def load_w(k):
                    wi, _, wrel = wring.get()
                    toks = []
                    for j in range(8):
                        toks.append(P.dma_fn("gpsimd", lambda e, wi=wi, j=j, k=k: gather_b(
                            e, wu[wi][:, j, :], wup_d.rearrange("l r d -> (l r) d"), IDXU[:, k * 8 + j:k * 8 + j + 1]), deps=wrel, stream=f"swu{wi}"))
                    for j in range(4):
                        toks.append(P.dma_fn("gpsimd", lambda e, wi=wi, j=j, k=k: gather_b(
                            e, wd[wi][:, j, :], wdn_d.rearrange("l r d -> (l r) d"), IDXD[:, k * 4 + j:k * 4 + j + 1]), deps=wrel, stream=f"swu{wi}"))
                    return wi, toks

                def load_x(k):
                    xi, xb, xrel = xring.get()
                    tx = P.dma("sync", xb[:], XS_d[k * TS:(k + 1) * TS, :].rearrange("(s p) d -> p s d", p=128), deps=[xrel], stream=f"sxt{xi}")
                    return xi, xb, tx

                def load_tile(k):
                    return load_w(k) + load_x(k)

                order = []
                lo_, hi_k = 0, KT - 1
                while lo_ <= hi_k:
                    order.append(lo_)
                    if hi_k != lo_:
                        order.append(hi_k)
                    lo_ += 1
                    hi_k -= 1
                wq = [load_w(order[0]), load_w(order[1])]
                xq = load_x(order[0])
                for kn, k in enumerate(order):
                    wi, wtoks = wq.pop(0)
                    xi, xb, tx = xq
                    if kn + 1 < KT:
                        xq = load_x(order[kn + 1])
                    if kn + 2 < KT:
                        wq.append(load_w(order[kn + 2]))
                    ti, xT_, trel = xTring.get()
                    tev = []
                    tlast = None
                    for half in range(4):
                        ui, banks, urel = upring.get()
                        bview = pbk[banks[0]].bitcast(BF16)
                        for q in range(8):
                            idx = half * 8 + q
                            sub, kc = idx // KC, idx % KC
                            tlast = P.op("tensor", lambda e, bview=bview, q=q, sub=sub, kc=kc, xb=xb: e.transpose(
                                bview[:, q * 128:(q + 1) * 128], xb[:, sub, kc * 128:(kc + 1) * 128], IDB),
                                deps=[tx, urel] + cdeps, sig=(q == 7))
                        sub0 = (half * 8) // KC
                        kc0 = (half * 8) % KC
                        dstv = xT_[:, kc0:kc0 + 8, sub0 * 128:(sub0 + 1) * 128]
                        srcv = bview.rearrange("p (a b) -> p a b", a=8)
                        if half % 2 == 0:
                            te = P.op("scalar", lambda e, dstv=dstv, srcv=srcv: e.copy(dstv, srcv), deps=[tlast, trel])
                        else:
                            te = P.op("vector", lambda e, dstv=dstv, srcv=srcv: e.tensor_copy(dstv, srcv), deps=[tlast, trel])
                        upring.release(ui, [te])
                        tev.append(te)
                    xring.release(xi, [tlast])
                    ai, ab_, arel = aring.get()
                    tms_last = None
                    acts = []
                    for i in range(4):
                        ui, banks, urel = upring.get()
                        tms = []
                        for n in range(2):
                            pb_ = pbk[banks[n]][:, 0:TS]
                            for kc in range(KC):
                                tm = P.op("tensor", lambda e, pb_=pb_, n=n, kc=kc, i=i, wi=wi, xT_=xT_: e.matmul(
                                    pb_, wu[wi][:, i * 2 + n, kc * 128:(kc + 1) * 128], xT_[:, kc, :],
                                    start=(kc == 0), stop=(kc == KC - 1)), deps=[wtoks, tev, urel], sig=(kc == KC - 1))
                            tms.append(tm)
                        tms_last = tm
                        si, sb_, srel = slring.get()
                        ta = P.op("scalar", lambda e, sb_=sb_, b0=pbk[banks[0]][:, 0:TS]: e.activation(sb_[:], b0, AF.Silu), deps=[tms[0], srel])
                        tb_ = P.op("vector", lambda e, ab_=ab_, i=i, sb_=sb_, b1=pbk[banks[1]][:, 0:TS]: e.tensor_tensor(ab_[:, i, :], sb_[:], b1, ALU.mult),
                                   deps=[ta, tms[1], arel])
                        slring.release(si, [tb_])
                        upring.release(ui, [ta, tb_])
                        acts.append(tb_)
                    xTring.release(ti, [tms_last])
                    tdl = None
                    for sub in range(2):
                        for d_ in range(2):
                            di, dh, drel = dnring.get()
                            yp = pall[:, dh * 1024:(dh + 1) * 1024]
                            for nb_ in range(2):
                                for c_ in range(4):
                                    tdl = P.op("tensor", lambda e, yp=yp, nb_=nb_, c_=c_, sub=sub, d_=d_, ab_=ab_, wi=wi: e.matmul(
                                        yp[:, nb_ * 512:(nb_ + 1) * 512], ab_[:, c_, sub * 128:(sub + 1) * 128],
                                        wd[wi][:, c_, d_ * 1024 + nb_ * 512:d_ * 1024 + (nb_ + 1) * 512], start=(c_ == 0), stop=(c_ == 3)),
                                        deps=[wtoks, acts, drel], sig=(nb_ == 1 and c_ == 3))
                            yi, yb, yrel = yring.get()
                            if (sub + d_) % 2 == 0:
                                tev_ = P.op("scalar", lambda e, yb=yb, yp=yp: e.copy(yb[:], yp), deps=[tdl, yrel])
                            else:
                                tev_ = P.op("vector", lambda e, yb=yb, yp=yp: e.tensor_copy(yb[:], yp), deps=[tdl, yrel])
                            dnring.release(di, [tev_])
                            tdo = P.dma("sync", YS_d[k * TS + sub * 128:k * TS + (sub + 1) * 128, d_ * 1024:(d_ + 1) * 1024], yb[:],
                                        deps=[tev_], stream=f"syst{yi}")
                            yring.release(yi, [tdo])
                    aring.release(ai, [tdl])
                    wring.release(wi, [tdl])
            with P.phase():
                yg = [P.sbuf(f"syg{i}", [128, 2, D], F32) for i in range(2)]
                gring = Ring(yg)
                ln = LNEpilogue(l, 1, res_src_d, res_dst_d, store_q="sync")

                def gath(tt):
                    gi, gb_, grel = gring.get()
                    t1_ = P.dma_fn("gpsimd", lambda e, gb_=gb_, tt=tt: e.indirect_dma_start(
                        out=gb_[:, 0, :], out_offset=None, in_=YS_d[:, :], in_offset=IOA(ap=SHI[:, tt:tt + 1], axis=0)), deps=grel, stream=f"syg{gi}")
                    t2_ = P.dma_fn("gpsimd", lambda e, gb_=gb_, tt=tt: e.indirect_dma_start(
                        out=gb_[:, 1, :], out_offset=None, in_=YS_d[:, :], in_offset=IOA(ap=SLO[:, tt:tt + 1], axis=0)), deps=grel, stream=f"syg{gi}")
                    return gi, gb_, [t1_, t2_]

                pg_ = gath(0)
                ln.prefetch(0)
                for tt in range(NT):
                    gi, gb_, gt = pg_
                    if tt + 1 < NT:
                        pg_ = gath(tt + 1)
                        ln.prefetch(tt + 1)
                    ta = P.op("scalar", lambda e, gb_=gb_, tt=tt: e.activation(gb_[:, 0, :], gb_[:, 0, :], AF.Identity, scale=GHI[:, tt:tt + 1]), deps=[gt])
                    tb_ = P.op(V, lambda e, gb_=gb_, tt=tt: e.scalar_tensor_tensor(gb_[:, 1, :], gb_[:, 1, :], GLO[:, tt:tt + 1], gb_[:, 0, :], ALU.mult, ALU.add), deps=[ta, gt])
                    tz = ln.run(tt, gb_[:, 1, :], [tb_])
                    gring.release(gi, [tz])
                if final:
                    out_toks.extend(ln.toks)

    def run_all():
        src = x_d
        for l in range(n_layers):
            phase_prep(src, l, router=False)
            if l == 0:
                dbg_dump("xT", xT[:])
            if stop_after == "prep":
                return
            with P.phase():
                phase_mixer(l)
            if stop_after in ("attn", "merge"):
                return
            phase_mixer_out(l, src, R_d[0] if l == 0 else R_d[2])
            src = R_d[0] if l == 0 else R_d[2]
            if l == 0:
                dbg_dump("h1", src)
            if stop_after == "ln1":
                return
            phase_prep(src, l, router=True)
            if l == 0:
                dbg_dump("gates", gates[:])
            if stop_after == "gates":
                return
            final = (l == n_layers - 1)
            dst = out_d if final else R_d[1]
            if SPARSE_MOE:
                phase_moe_sparse(l, src, dst, final)
            else:
                with P.phase():
                    phase_moe(l, src, dst, final)
            if stop_after in ("moe_y",):
                return
            src = dst

    run_all()
    if stop_after is not None and stop_after not in ("prep", "gates"):
        pass
    P.base = []
    P.finish_wait("sync", [out_toks, P.barrier()])
    P.emit()
    P.close()
    return nc


def make_consts():
    c = np.zeros((128, NCONST), np.float32)
    i = np.arange(128)
    key = i[:, None]
    qry = i[None, :]
    c[:, 0:128] = (key <= qry)
    c[:, 128:256] = (key >= qry)
    c[:, 256:384] = (key < qry)
    c[:, 384:512] = -1.0 * (key >= qry)
    c[:, 512:640] = -1.0
    c[:, 640:768] = 1.0
    perm = np.zeros((32, 32), np.float32)
    for m in range(32):
        perm[(m + 16) % 32, m] = 1.0
    c[0:32, 768:800] = perm
    c[:, 896:1024] = np.eye(128, dtype=np.float32)
    cs = np.zeros((128, 64), np.float32)
    cs[:, 8:40] = np.arange(32, dtype=np.float32)[None, :]
    cs[:, 40:48] = (np.arange(8)[None, :] * 128 + np.arange(128)[:, None]).astype(np.float32)
    half = 16
    inv_freq = (np.float32(500000.0) ** (np.float32(-2.0) * np.arange(half, dtype=np.float32) / np.float32(32))).astype(np.float32)
    cs[0:16, 0] = inv_freq
    cs[16:32, 0] = inv_freq
    cs[0:16, 1] = -1.0
    cs[16:32, 1] = 1.0
    return c, cs


def prep_inputs(x, positions, w_in, p_a, p_b, w_o, ln_g, ln_b, router_coarse, router_coarse_bias,
                router_fine, router_fine_bias, w_up, w_down):
    f = np.float32
    w_in_r = np.ascontiguousarray(np.asarray(w_in, f).reshape(L, KC, 128, 92, 128).transpose(0, 3, 2, 1, 4))
    pa_r = np.ascontiguousarray(np.asarray(p_a, f).reshape(L, 4, 128, 16, 128).transpose(0, 3, 2, 1, 4))
    pb_r = np.ascontiguousarray(np.asarray(p_b, f).reshape(L, 8, 128, 16, 128).transpose(0, 3, 2, 1, 4))
    wo_r = np.ascontiguousarray(np.asarray(w_o, f).reshape(L, KC, 128, D).transpose(0, 2, 1, 3))
    lng = np.stack([np.asarray(ln_g, f), np.asarray(ln_b, f)], axis=2)
    lng = np.ascontiguousarray(np.broadcast_to(lng[:, :, :, None, :], (L, 2, 2, 128, D)))
    wr = np.concatenate([np.asarray(router_coarse, f), np.asarray(router_fine, f)], axis=2)
    wr_r = np.ascontiguousarray(wr.reshape(L, KC, 128, 20).transpose(0, 2, 1, 3))
    rb = np.concatenate([np.asarray(router_coarse_bias, f), np.asarray(router_fine_bias, f)], axis=1)
    rb_r = np.ascontiguousarray(np.broadcast_to(rb[:, None, :], (L, 128, 20)))
    wu = np.asarray(w_up, f).reshape(L, 16, KC, 128, 2, 4, 128)
    wu_r = np.ascontiguousarray(wu.transpose(0, 1, 5, 4, 3, 2, 6)).reshape(L, 16 * 4 * 2 * 128, 2048)
    wd = np.ascontiguousarray(np.asarray(w_down, f)).reshape(L, 16 * 512, D)
    consts, cscal = make_consts()
    shared = {"w_in": w_in_r, "p_a": pa_r, "p_b": pb_r, "w_o": wo_r, "ln_gb": lng, "w_r": wr_r, "r_b": rb_r,
              "w_up": wu_r, "w_down": wd, "consts": consts, "cscal": cscal}
    x = np.asarray(x, f)
    positions = np.asarray(positions, np.int32)
    in_maps = []
    for b in range(NCORES):
        m = dict(shared)
        m["x"] = np.ascontiguousarray(x[b])
        m["pos"] = np.ascontiguousarray(np.broadcast_to(positions[b][None, :], (32, S)))
        in_maps.append(m)
    return in_maps


_NC_CACHE = {}


def kernel(x, positions, w_in, p_a, p_b, w_o, ln_g, ln_b, router_coarse, router_coarse_bias,
           router_fine, router_fine_bias, w_up, w_down):
    in_maps = prep_inputs(x, positions, w_in, p_a, p_b, w_o, ln_g, ln_b, router_coarse, router_coarse_bias,
                          router_fine, router_fine_bias, w_up, w_down)
    if "nc" not in _NC_CACHE:
        _NC_CACHE["nc"] = build_nc()
    nc = _NC_CACHE["nc"]
    res = run_bass_kernel_spmd(nc, in_maps, core_ids=list(range(NCORES)))
    out = np.stack([np.asarray(r["out"], np.float32) for r in res.results], axis=0)
    return out
```

```python
import numpy as np
import concourse.bass as bass
import concourse.mybir as mybir
from concourse.bass_utils import run_bass_kernel_spmd

F32 = mybir.dt.float32
BF16 = mybir.dt.bfloat16
I32 = mybir.dt.int32
AF = mybir.ActivationFunctionType
ALU = mybir.AluOpType
AX = mybir.AxisListType

ENGINES = ("sync", "scalar", "gpsimd", "vector", "tensor")

S = 2048
D = 2048
KC = 16
NT = 16
L = 2
NCORES = 8
ALPHA = float((2 * L) ** 0.25)
LN_EPS = 1e-5
SCALE = float(128 ** -0.5)
DILS = (1, 4, 16)
PI = float(np.pi)
NCONST = 128 * 8
SPARSE_MOE = True


class Prog:
    def __init__(self, nc):
        self.nc = nc
        self.q = {e: [] for e in ENGINES}
        self.cnt = {}
        self.sems = {}
        self.waited = {}
        self._ctx = []
        self.base = []
        for e in ENGINES:
            self._sem("E_" + e)

    def _sem(self, name):
        if name not in self.sems:
            cm = self.nc.semaphore(name)
            self.sems[name] = cm.__enter__()
            self.cnt[name] = 0
        return self.sems[name]

    def sbuf(self, name, shape, dt):
        self._uid = getattr(self, "_uid", 0) + 1
        cm = self.nc.sbuf_tensor(f"sb{self._uid}_{name}", list(shape), dt)
        t = cm.__enter__()
        self._ctx.append(cm)
        return t

    def psum(self, name, shape, dt):
        cm = self.nc.psum_tensor(name, list(shape), dt)
        t = cm.__enter__()
        self._ctx.append(cm)
        return t

    def _flat(self, deps, out):
        for tok in deps:
            if tok is None:
                continue
            if isinstance(tok, tuple) and len(tok) == 2 and isinstance(tok[0], str):
                out.append(tok)
            else:
                self._flat(tok, out)

    def _waits(self, eng, deps):
        flat = []
        self._flat(deps, flat)
        self._flat(self.base, flat)
        mx = {}
        for sname, val in flat:
            if mx.get(sname, 0) < val:
                mx[sname] = val
        waits = []
        for sname, val in mx.items():
            key = (eng, sname)
            if self.waited.get(key, 0) < val:
                self.waited[key] = val
                waits.append((sname, val))
        return waits

    def op(self, eng, fn, deps=(), sig=True):
        waits = self._waits(eng, deps)
        tok = None
        sname = None
        if sig:
            sname = "E_" + eng
            self.cnt[sname] += 1
            tok = (sname, self.cnt[sname])
        self.q[eng].append((fn, waits, sname, 1))
        return tok

    def dma(self, eng, out, in_, deps=(), stream=None):
        waits = self._waits(eng, deps)
        sname = "D_" + (stream or eng)
        self._sem(sname)
        self.cnt[sname] += 16
        tok = (sname, self.cnt[sname])
        self.q[eng].append((lambda e: e.dma_start(out=out, in_=in_), waits, sname, 16))
        return tok

    def dma_fn(self, eng, fn, deps=(), stream=None):
        waits = self._waits(eng, deps)
        sname = "D_" + (stream or eng)
        self._sem(sname)
        self.cnt[sname] += 16
        tok = (sname, self.cnt[sname])
        self.q[eng].append((fn, waits, sname, 16))
        return tok

    def finish_wait(self, eng, deps):
        waits = self._waits(eng, deps)
        self.q[eng].append((None, waits, None, 0))

    def barrier(self):
        return [(n, c) for n, c in self.cnt.items() if c > 0]

    class _Phase:
        def __init__(self, P):
            self.P = P

        def __enter__(self):
            self.depth = len(self.P._ctx)
            self.P.base = self.P.barrier()
            return self

        def __exit__(self, *a):
            P = self.P
            while len(P._ctx) > self.depth:
                P._ctx.pop().__exit__(None, None, None)
            P.base = P.barrier()
            return False

    def phase(self):
        return Prog._Phase(self)

    def emit(self):
        nc = self.nc
        with nc.Block() as block:
            def mk(ename):
                def body(e):
                    for fn, waits, sname, inc in self.q[ename]:
                        for (wn, wv) in waits:
                            e.wait_ge(self.sems[wn], wv)
                        if fn is None:
                            continue
                        ins = fn(e)
                        if sname is not None:
                            ins.then_inc(self.sems[sname], inc)
                return body
            block.sync(mk("sync"))
            block.scalar(mk("scalar"))
            block.gpsimd(mk("gpsimd"))
            block.vector(mk("vector"))
            block.tensor(mk("tensor"))

    def close(self):
        while self._ctx:
            self._ctx.pop().__exit__(None, None, None)


class Ring:
    def __init__(self, bufs):
        self.bufs = list(bufs)
        self.i = 0
        self.rel = [[] for _ in self.bufs]

    def get(self):
        idx = self.i % len(self.bufs)
        self.i += 1
        return idx, self.bufs[idx], self.rel[idx]

    def release(self, idx, toks):
        self.rel[idx] = list(toks)


def build_nc(n_layers=L, stop_after=None, dbg=()):
    nc = bass.Bass("TRN2", target_bir_lowering=False)

    def din(name, shape, dt=F32):
        return nc.dram_tensor(name, list(shape), dt, kind="ExternalInput").ap()

    x_d = din("x", [S, D])
    pos_d = din("pos", [32, S], I32)
    win_d = din("w_in", [L, 92, 128, KC, 128])
    pa_d = din("p_a", [L, 16, 128, 4, 128])
    pb_d = din("p_b", [L, 16, 128, 8, 128])
    wo_d = din("w_o", [L, 128, KC, D])
    lng_d = din("ln_gb", [L, 2, 2, 128, D])
    wr_d = din("w_r", [L, 128, KC, 20])
    rb_d = din("r_b", [L, 128, 20])
    wup_d = din("w_up", [L, 16 * 4 * 2 * 128, 2048])
    wdn_d = din("w_down", [L, 16 * 512, D])
    KT = 32
    TS = 256
    XS_d = nc.dram_tensor("xs_scr", [KT * TS, D], BF16).ap()
    YS_d = nc.dram_tensor("ys_scr", [KT * TS, D], F32).ap()
    cst_d = din("consts", [128, NCONST])
    csc_d = din("cscal", [128, 64])
    out_d = nc.dram_tensor("out", [S, D], F32, kind="ExternalOutput").ap()
    R_d = [nc.dram_tensor(f"resid{i}", [S, D], F32).ap() for i in range(3)]
    mT_d = nc.dram_tensor("mT_scr", [NT, 128, KC, 128], BF16).ap()
    dbg_d = {}
    for name, shape in dbg:
        dbg_d[name] = nc.dram_tensor("dbg_" + name, list(shape), F32, kind="ExternalOutput").ap()

    P = Prog(nc)
    out_toks = []

    pall = P.psum("pall", [128, 8 * 512], F32)
    pbk = [pall[:, i * 512:(i + 1) * 512] for i in range(8)]
    cbf = P.sbuf("cbf", [128, NCONST], BF16)
    ident = P.sbuf("ident", [128, 128], F32)
    cscal = P.sbuf("cscal", [128, 64], F32)
    gates = P.sbuf("gates", [128, NT, 16], F32)
    selmask = P.sbuf("selmask", [128, NT, 16], F32)
    xT = P.sbuf("xT", [128, KC, S], BF16)
    M2 = cbf[:, 0:256]
    MSTRICT = cbf[:, 256:384]
    NEGU = cbf[:, 384:512]
    NEGONES = cbf[:, 512:640]
    ONES = cbf[:, 640:768]
    PERM = cbf[0:32, 768:800]

    t_c1 = P.dma("gpsimd", cbf[:], cst_d, stream="c1")
    t_c2 = P.dma("sync", ident[:], cst_d[:, 896:1024], stream="c2")
    t_c3 = P.dma("sync", cscal[:], csc_d, stream="c3")
    cdeps = [t_c1, t_c2, t_c3]

    def dbg_dump(name, src_ap, dst=None):
        if name in dbg_d:
            t = P.dma("gpsimd", dbg_d[name] if dst is None else dst, src_ap, deps=P.barrier(), stream="dbg")
            out_toks.append(t)

    rot_d = nc.dram_tensor("rot_scr", [2, 32, S], F32).ap()
    rot_tabs = {}
    with P.phase():
        cos_t = P.sbuf("cos_t0", [32, S], F32)
        sin_t = P.sbuf("sin_t0", [32, S], F32)
        posi = P.sbuf("posi", [32, S], I32)
        ang = P.sbuf("ang", [32, S], F32)
        twopi = P.sbuf("twopi", [32, S], F32)
        tmpa = P.sbuf("tmpa", [32, S], F32)
        t_p = P.dma("sync", posi[:], pos_d, stream="pos")
        t0 = P.op("vector", lambda e: e.tensor_copy(ang[:], posi[:]), deps=[t_p])
        t1 = P.op("vector", lambda e: e.tensor_scalar(ang[:], ang[:], cscal[0:32, 0:1], None, ALU.mult), deps=[t0] + cdeps)
        ki = P.sbuf("rot_ki", [32, S], I32)

        def sin_of(dst, shift, dep):
            d = P.op("vector", lambda e: e.tensor_scalar(tmpa[:], ang[:], shift, 1.0 / (2.0 * PI), ALU.add, ALU.mult), deps=[dep])
            d = P.op("vector", lambda e: e.tensor_copy(ki[:], tmpa[:]), deps=[d])
            d = P.op("vector", lambda e: e.tensor_copy(twopi[:], ki[:]), deps=[d])
            d = P.op("vector", lambda e: e.tensor_scalar(tmpa[:], ang[:], shift, None, ALU.add), deps=[d])
            d = P.op("vector", lambda e: e.scalar_tensor_tensor(tmpa[:], twopi[:], -2.0 * PI, tmpa[:], ALU.mult, ALU.add), deps=[d])
            d = P.op("vector", lambda e: e.tensor_scalar(twopi[:], tmpa[:], PI, None, ALU.is_gt), deps=[d])
            d = P.op("vector", lambda e: e.scalar_tensor_tensor(tmpa[:], twopi[:], -2.0 * PI, tmpa[:], ALU.mult, ALU.add), deps=[d])
            d = P.op("vector", lambda e: e.tensor_scalar(twopi[:], tmpa[:], -PI, None, ALU.is_lt), deps=[d])
            d = P.op("vector", lambda e: e.scalar_tensor_tensor(tmpa[:], twopi[:], 2.0 * PI, tmpa[:], ALU.mult, ALU.add), deps=[d])
            d = P.op("vector", lambda e: e.tensor_scalar(tmpa[:], tmpa[:], PI, -PI, ALU.min, ALU.max), deps=[d])
            d = P.op("scalar", lambda e: e.activation(dst[:], tmpa[:], AF.Sin), deps=[d])
            return d

        t4 = sin_of(sin_t, 0.0, t1)
        t5 = P.op("vector", lambda e: e.tensor_scalar(sin_t[:], sin_t[:], cscal[0:32, 1:2], None, ALU.mult), deps=[t4])
        t8 = sin_of(cos_t, PI / 2, t4)
        P.dma("sync", rot_d[0], cos_t[:], deps=[t8], stream="rot")
        P.dma("sync", rot_d[1], sin_t[:], deps=[t5], stream="rot")

    def phase_prep(src_d, l, router):
        with P.phase():
            xs = [P.sbuf(f"xs{i}", [128, D], F32) for i in range(2)]
            ring = Ring(xs)
            pring = Ring([0, 1, 2, 3])
            if router:
                wr = P.sbuf("wr", [128, KC, 20], F32)
                rb = P.sbuf("rb", [128, 20], F32)
                h32s = [P.sbuf(f"h32_{i}", [128, KC, 128], F32) for i in range(2)]
                hring = Ring(h32s)
                LG = P.sbuf("LG", [128, NT, 20], F32)
                lg_toks = []
                t_wr = P.dma("sync", wr[:], wr_d[l], stream="wr")
                t_rb = P.dma("sync", rb[:], rb_d[l], stream="rb")
                lring = Ring([4, 5])
            if not router:
                xb16 = [P.sbuf(f"xb16_{i}", [128, D], BF16) for i in range(2)]
                b16ring = Ring(xb16)
                IDB_ = cbf[:, 896:1024]
            for tt in range(NT):
                i, buf, rel = ring.get()
                t_ld = P.dma("sync", buf[:], src_d[tt * 128:(tt + 1) * 128, :], deps=rel, stream=f"xs{i}")
                if not router:
                    bi_, b16, brel_ = b16ring.get()
                    if tt % 2 == 0:
                        tc_ = P.op("scalar", lambda e, b16=b16, buf=buf: e.copy(b16[:], buf[:]), deps=[t_ld, brel_])
                    else:
                        tc_ = P.op("vector", lambda e, b16=b16, buf=buf: e.tensor_copy(b16[:], buf[:]), deps=[t_ld, brel_])
                    ring.release(i, [tc_])
                    tk = None
                    for g in range(2):
                        pi_, bk, prel = pring.get()
                        bview = pbk[bk].bitcast(BF16)
                        for j in range(8):
                            kc = g * 8 + j
                            tk = P.op("tensor", lambda e, bview=bview, j=j, kc=kc, b16=b16: e.transpose(
                                bview[:, j * 128:(j + 1) * 128], b16[:, kc * 128:(kc + 1) * 128], IDB_),
                                deps=[tc_, prel] + cdeps, sig=(j == 7))
                        src_v = bview.rearrange("p (a b) -> p a b", a=8)
                        dst_v = xT[:, g * 8:(g + 1) * 8, tt * 128:(tt + 1) * 128]
                        if (g + tt) % 2 == 0:
                            te = P.op("vector", lambda e, d=dst_v, s=src_v: e.tensor_copy(d, s), deps=[tk])
                        else:
                            te = P.op("scalar", lambda e, d=dst_v, s=src_v: e.copy(d, s), deps=[tk])
                        pring.release(pi_, [te])
                    b16ring.release(bi_, [tk])
                    continue
                if router:
                    hi, h32, hrel = hring.get()
                tks = []
                h32w = []
                for g in range(4):
                    pi_, bk, prel = pring.get()
                    pb_ = pbk[bk]
                    tk = None
                    for j in range(4):
                        kc = g * 4 + j
                        tk = P.op("tensor", lambda e, pb_=pb_, j=j, kc=kc, buf=buf: e.transpose(
                            pb_[:, j * 128:(j + 1) * 128], buf[:, kc * 128:(kc + 1) * 128], ident[:]),
                            deps=[t_ld, prel] + cdeps, sig=(j == 3))
                    tks.append(tk)
                    src_v = pb_.rearrange("p (a b) -> p a b", a=4)
                    dst_v = xT[:, g * 4:(g + 1) * 4, tt * 128:(tt + 1) * 128]
                    if not router:
                        if g % 2 == 0:
                            te = P.op("scalar", lambda e, d=dst_v, s=src_v: e.copy(d, s), deps=[tk])
                        else:
                            te = P.op("vector", lambda e, d=dst_v, s=src_v: e.tensor_copy(d, s), deps=[tk])
                        rels = [te]
                    else:
                        d2 = h32[:, g * 4:(g + 1) * 4, :]
                        if g % 2 == 0:
                            te2 = P.op("vector", lambda e, d=d2, s=src_v: e.tensor_copy(d, s), deps=[tk, hrel])
                        else:
                            te2 = P.op("scalar", lambda e, d=d2, s=src_v: e.copy(d, s), deps=[tk, hrel])
                        rels = [te2]
                        h32w.append(te2)
                        if not SPARSE_MOE:
                            te = P.op("gpsimd", lambda e, d=dst_v, s=d2: e.tensor_copy(d, s), deps=[te2])
                            h32w.append(te)
                    pring.release(pi_, rels)
                ring.release(i, tks)
                if router:
                    li, lbk, lrel = lring.get()
                    lp = pbk[lbk][:, 0:20]
                    tm = None
                    for kc in range(KC):
                        tm = P.op("tensor", lambda e, lp=lp, h32=h32, kc=kc: e.matmul(
                            lp, h32[:, kc, :], wr[:, kc, :], start=(kc == 0), stop=(kc == KC - 1)),
                            deps=[h32w, t_wr, lrel], sig=(kc == KC - 1))
                    hring.release(hi, [tm] + h32w)
                    ta_ = P.op("vector", lambda e, lp=lp, tt=tt: e.tensor_tensor(LG[:, tt, :], lp, rb[:], ALU.add), deps=[tm, t_rb])
                    lring.release(li, [ta_])
                    lg_toks.append(ta_)
            if router:
                V = "vector"
                T16 = NT

                def W(name, n):
                    return P.sbuf("g_" + name, [128, T16 * n], F32)

                def v3(t, n):
                    return t[:].rearrange("p (t n) -> p t n", t=T16)

                def bc(t, n):
                    return t[:].rearrange("p (t o) -> p t o", o=1).broadcast_to([128, T16, n])

                mx, dd, ex, sme, pg, oh4 = W("mx", 1), W("dd", 4), W("ex", 4), W("sme", 1), W("pg", 1), W("oh4", 4)
                prod, sel, m1, d1, oh1, sel2 = W("prod", 16), W("sel", 4), W("m1", 1), W("d1", 4), W("oh1", 4), W("sel2", 4)
                m2, d2, oh2, dm, ed, w1, w2, wi1, wi2 = W("m2", 1), W("d2", 4), W("oh2", 4), W("dm", 1), W("ed", 1), W("w1", 1), W("w2", 1), W("wi1", 4), W("wi2", 4)
                lc = LG[:, :, 0:4]
                lf = LG[:, :, 4:20]
                c = P.op(V, lambda e: e.reduce_max(mx[:], lc, AX.X), deps=[lg_toks])
                c = P.op(V, lambda e: e.tensor_tensor(v3(dd, 4), lc, bc(mx, 4), ALU.subtract), deps=[c])
                cx = P.op("scalar", lambda e: e.activation(ex[:], dd[:], AF.Exp), deps=[c])
                cx = P.op(V, lambda e: e.reduce_sum(sme[:], v3(ex, 4), AX.X), deps=[cx])
                cx = P.op(V, lambda e: e.reciprocal(pg[:], sme[:]), deps=[cx])
                c = P.op(V, lambda e: e.tensor_scalar(oh4[:], dd[:], 0.0, None, ALU.is_equal), deps=[c])
                p4 = prod[:].rearrange("p (t g e) -> p t g e", t=T16, g=4)
                lf4 = lf.rearrange("p t (g e) -> p t g e", g=4)
                oh4b = oh4[:].rearrange("p (t g o) -> p t g o", t=T16, o=1).broadcast_to([128, T16, 4, 4])
                c = P.op(V, lambda e: e.tensor_tensor(p4, lf4, oh4b, ALU.mult), deps=[c])
                c = P.op(V, lambda e: e.tensor_reduce(v3(sel, 4), prod[:].rearrange("p (t g e) -> p t e g", t=T16, g=4), AX.X, ALU.add), deps=[c])
                c = P.op(V, lambda e: e.reduce_max(m1[:], v3(sel, 4), AX.X), deps=[c])
                c = P.op(V, lambda e: e.tensor_tensor(v3(d1, 4), v3(sel, 4), bc(m1, 4), ALU.subtract), deps=[c])
                c = P.op(V, lambda e: e.tensor_scalar(oh1[:], d1[:], 0.0, None, ALU.is_equal), deps=[c])
                c = P.op(V, lambda e: e.scalar_tensor_tensor(sel2[:], oh1[:], -1e30, sel[:], ALU.mult, ALU.add), deps=[c])
                c = P.op(V, lambda e: e.reduce_max(m2[:], v3(sel2, 4), AX.X), deps=[c])
                c = P.op(V, lambda e: e.tensor_tensor(v3(d2, 4), v3(sel2, 4), bc(m2, 4), ALU.subtract), deps=[c])
                c = P.op(V, lambda e: e.tensor_scalar(oh2[:], d2[:], 0.0, None, ALU.is_equal), deps=[c])
                c = P.op(V, lambda e: e.tensor_tensor(dm[:], m2[:], m1[:], ALU.subtract), deps=[c])
                c = P.op("scalar", lambda e: e.activation(ed[:], dm[:], AF.Exp), deps=[c])
                c = P.op(V, lambda e: e.tensor_scalar(w1[:], ed[:], 1.0, None, ALU.add), deps=[c])
                c = P.op(V, lambda e: e.reciprocal(w1[:], w1[:]), deps=[c])
                c = P.op(V, lambda e: e.tensor_tensor(w2[:], ed[:], w1[:], ALU.mult), deps=[c])
                c = P.op(V, lambda e: e.tensor_tensor(w1[:], w1[:], pg[:], ALU.mult), deps=[c, cx])
                c = P.op(V, lambda e: e.tensor_tensor(w2[:], w2[:], pg[:], ALU.mult), deps=[c])
                c = P.op(V, lambda e: e.tensor_tensor(v3(wi1, 4), v3(oh1, 4), bc(w1, 4), ALU.mult), deps=[c])
                c = P.op(V, lambda e: e.tensor_tensor(v3(wi2, 4), v3(oh2, 4), bc(w2, 4), ALU.mult), deps=[c])
                c = P.op(V, lambda e: e.tensor_tensor(wi1[:], wi1[:], wi2[:], ALU.add), deps=[c])
                g4 = gates[:].rearrange("p t (g e) -> p t g e", g=4)
                wib = wi1[:].rearrange("p (t o e) -> p t o e", t=T16, o=1).broadcast_to([128, T16, 4, 4])
                c = P.op(V, lambda e: e.tensor_tensor(g4, oh4b, wib, ALU.mult), deps=[c])
                c = P.op(V, lambda e: e.tensor_tensor(wi2[:], oh1[:], oh2[:], ALU.add), deps=[c])
                sm4 = selmask[:].rearrange("p t (g e) -> p t g e", g=4)
                w2b = wi2[:].rearrange("p (t o e) -> p t o e", t=T16, o=1).broadcast_to([128, T16, 4, 4])
                c = P.op(V, lambda e: e.tensor_tensor(sm4, oh4b, w2b, ALU.mult), deps=[c])

    def inproj_fm(wsl, dst, scale, evac_eng, bring, extra_deps, rotary, tbs=None):
        toks = []
        pending_rot = None

        def emit_rot(tb, te):
            rbank, rtmp, rt2, rstate = rotary
            rp = pbk[rbank][0:32, :]
            d32 = dst[0:32, tb * 512:(tb + 1) * 512]
            tp = P.op("tensor", lambda e: e.matmul(rp, PERM, d32, start=True, stop=True),
                      deps=[te, rstate["rel"]] + cdeps)
            ta = P.op("vector", lambda e: e.tensor_tensor(
                rtmp[:], rp, rot_tabs["sin"][:, tb * 512:(tb + 1) * 512], ALU.mult), deps=[tp, rstate["tmp_rel"], rot_tabs["tok"]])
            rstate["rel"] = [ta]
            tb2 = P.op("gpsimd", lambda e: e.tensor_tensor(
                rt2[:], d32, rot_tabs["cos"][:, tb * 512:(tb + 1) * 512], ALU.mult), deps=[te, rstate["tmp_rel"], rot_tabs["tok"]])
            tcomb = P.op("vector", lambda e: e.tensor_tensor(d32, rtmp[:], rt2[:], ALU.add), deps=[ta, tb2, tp])
            rstate["tmp_rel"] = [tcomb]
            return tcomb

        for tb in (range(4) if tbs is None else tbs):
            bi, bk, brel = bring.get()
            pb_ = pbk[bk]
            tm = None
            for kc in range(KC):
                tm = P.op("tensor", lambda e, pb_=pb_, kc=kc, tb=tb: e.matmul(
                    pb_, wsl[:, kc, :], xT[:, kc, tb * 512:(tb + 1) * 512], start=(kc == 0), stop=(kc == KC - 1)),
                    deps=[extra_deps, brel], sig=(kc == KC - 1))
            dv = dst[:, tb * 512:(tb + 1) * 512]
            if evac_eng == "scalar":
                if scale is None:
                    te = P.op("scalar", lambda e, dv=dv, pb_=pb_: e.copy(dv, pb_), deps=[tm, extra_deps])
                else:
                    te = P.op("scalar", lambda e, dv=dv, pb_=pb_: e.mul(dv, pb_, scale), deps=[tm, extra_deps])
            else:
                if scale is None:
                    te = P.op("vector", lambda e, dv=dv, pb_=pb_: e.tensor_copy(dv, pb_), deps=[tm, extra_deps])
                else:
                    te = P.op("vector", lambda e, dv=dv, pb_=pb_: e.tensor_scalar(dv, pb_, scale, None, ALU.mult), deps=[tm, extra_deps])
            bring.release(bi, [te])
            if rotary is not None:
                if pending_rot is not None:
                    toks.append(emit_rot(*pending_rot))
                pending_rot = (tb, te)
            else:
                toks.append(te)
        if rotary is not None and pending_rot is not None:
            toks.append(emit_rot(*pending_rot))
        return toks

    def tok_slice(dl, r, blk, n=1):
        start = r + dl * 128 * blk
        return slice(start, start + dl * (128 * n - 1) + 1, dl)

    def phase_mixer(l):
        oAT = P.sbuf("oAT", [128, 4, S], BF16)
        oBT = P.sbuf("oBT", [128, 8, S], BF16)
        for which in ("A", "B"):
            with P.phase():
                wsl = [P.sbuf(f"wsl{i}", [128, 3, KC, 128], BF16) for i in range(2)]
                wring = Ring(wsl)
                qk = [P.sbuf(f"qk{i}", [128, 2, S], BF16) for i in range(2)]
                qkring = Ring(qk)
                vv = [P.sbuf(f"vv{i}", [128, NT, 128], BF16) for i in range(2)]
                vring = Ring(vv)
                if which == "A":
                    ND = P.sbuf("ND", [128, 2, S], F32)
                    rot_tabs["cos"] = P.sbuf("cos_t", [32, S], F32)
                    rot_tabs["sin"] = P.sbuf("sin_t", [32, S], F32)
                    rot_tabs["tok"] = [P.dma("sync", rot_tabs["cos"][:], rot_d[0], stream="rotl"),
                                       P.dma("sync", rot_tabs["sin"][:], rot_d[1], stream="rotl")]
                    rtmp = P.sbuf("rtmp", [32, 512], F32)
                    rt2 = P.sbuf("rt2", [32, 512], F32)
                    pT = [P.sbuf(f"pT{i}", [128, 512], BF16) for i in range(3)]
                    pTring = Ring(pT)
                bring = Ring([0, 1, 2])
                rstate = {"rel": [], "tmp_rel": []}
                string = Ring([4, 5])
                pvring = Ring([6, 7])
                nd_rel = []

                if which == "A":
                    heads = [("A", g, j) for j in range(4) for g in range(3)]
                else:
                    heads = [("B", h, 0) for h in range(8)]

                def load_w(hd):
                    kind, a, b_ = hd
                    wi, wbuf, wrel = wring.get()
                    if kind == "A":
                        cq, ck, cv = a * 4 + b_, 12 + a * 4 + b_, 24 + a * 4 + b_
                    else:
                        cq, ck, cv = 36 + a, 44 + a, 52 + a
                    toks = []
                    for n, c in enumerate((cq, ck, cv)):
                        toks.append(P.dma("gpsimd", wbuf[:, n], win_d[l, c], deps=wrel, stream=f"wsl{wi}"))
                    return wi, wbuf, toks

                pending = load_w(heads[0])
                bstate = {}
                if which == "B":
                    ebuf = [P.sbuf(f"ebuf{i}", [128, 512], F32) for i in range(2)]
                    ering = Ring(ebuf)
                    spb = [P.sbuf(f"spb{i}", [128, 512], BF16) for i in range(3)]
                    cbuf = [P.sbuf(f"cbuf{i}", [128, 512], BF16) for i in range(3)]
                    abuf = [P.sbuf(f"abuf{i}", [128, 512], BF16) for i in range(3)]
                zring = Ring([3, 4, 5])
                oring = Ring([6, 7])

                for hi_, hd in enumerate(heads):
                    kind, a, b_ = hd
                    wi, wbuf, wtoks = pending
                    if hi_ + 1 < len(heads):
                        pending = load_w(heads[hi_ + 1])
                    if kind == "A":
                        qi, qkb, qrel = qkring.get()
                        vi, vb, vrel = vring.get()
                        qT = qkb[:, 0, :]
                        kT = qkb[:, 1, :]
                    if kind == "A":
                        g, j = a, b_
                        dl = DILS[g]
                        nb = 16 // dl
                        rot = (3, rtmp, rt2, rstate)
                        tq = inproj_fm(wbuf[:, 0], qT, SCALE, "scalar", bring, [wtoks, qrel], rot)
                        tk_ = inproj_fm(wbuf[:, 1], kT, None, "scalar", bring, [wtoks, qrel], rot)
                        tv = []
                        for t4 in range(4):
                            bi, bk, brel = bring.get()
                            pb_ = pbk[bk]
                            tm = None
                            for tq4 in range(4):
                                tau = t4 * 4 + tq4
                                r, blk = tau // nb, tau % nb
                                sl = tok_slice(dl, r, blk)
                                for kc in range(KC):
                                    tm = P.op("tensor", lambda e, pb_=pb_, tq4=tq4, kc=kc, sl=sl, wbuf=wbuf: e.matmul(
                                        pb_[:, tq4 * 128:(tq4 + 1) * 128], xT[:, kc, sl], wbuf[:, 2, kc, :],
                                        start=(kc == 0), stop=(kc == KC - 1)),
                                        deps=[wtoks, brel], sig=(tq4 == 3 and kc == KC - 1))
                            dv = vb[:, t4 * 4:(t4 + 1) * 4, :]
                            sv = pb_.rearrange("p (a b) -> p a b", a=4)
                            te = P.op("vector", lambda e, dv=dv, sv=sv: e.tensor_copy(dv, sv), deps=[tm, vrel])
                            bring.release(bi, [te])
                            tv.append(te)
                        wring.release(wi, [tm])
                        last_pe = None
                        tev = []

                        def emit_front(t2_, dl=dl, nb=nb, qT=qT, kT=kT, tq=tq, tk_=tk_):
                            taus = [2 * t2_, 2 * t2_ + 1]
                            si, sbk, srel = string.get()
                            sp_ = pbk[sbk]
                            col = 0
                            layout = []
                            specs = []
                            for tau in taus:
                                r, blk = tau // nb, tau % nb
                                parts = [(r, blk)] + ([(r, blk - 1)] if blk > 0 else [])
                                qsl = tok_slice(dl, r, blk)
                                pc = []
                                for (pr, pblk) in parts:
                                    specs.append((col, tok_slice(dl, pr, pblk), qsl))
                                    pc.append((col, pr * nb + pblk))
                                    col += 128
                                layout.append((tau, qsl, pc))
                            ts = None
                            for n_, (col_, ksl, qsl_) in enumerate(specs):
                                ts = P.op("tensor", lambda e, sp_=sp_, col_=col_, ksl=ksl, qsl_=qsl_, kT=kT, qT=qT: e.matmul(
                                    sp_[:, col_:col_ + 128], kT[:, ksl], qT[:, qsl_], start=True, stop=True),
                                    deps=[tq, tk_, srel], sig=(n_ == len(specs) - 1))
                            pi2, pTb, prel = pTring.get()
                            texp = P.op("scalar", lambda e, pTb=pTb, sp_=sp_, col=col: e.activation(
                                pTb[:, 0:col], sp_[:, 0:col], AF.Exp), deps=[ts, prel])
                            string.release(si, [texp])
                            tmk = []
                            for (tau, qsl, pc) in layout:
                                c0 = pc[0][0]
                                w_ = 128 * len(pc)
                                tmk.append(P.op("gpsimd", lambda e, pTb=pTb, c0=c0, w_=w_: e.tensor_tensor(
                                    pTb[:, c0:c0 + w_], pTb[:, c0:c0 + w_], M2[:, 0:w_], ALU.mult), deps=[texp] + cdeps))
                            return layout, pi2, pTb, tmk

                        def emit_back(ctx, g=g, vb=vb, tv=tv):
                            layout, pi2, pTb, tmk = ctx
                            vi2, vbk, vrel2 = pvring.get()
                            pv_ = pbk[vbk]
                            tpv = None
                            for n, (tau, qsl, pc) in enumerate(layout):
                                for wh in range(2):
                                    for m, (c_, ktau) in enumerate(pc):
                                        lhs = vb[:, ktau, :] if wh == 0 else ONES
                                        o_ = pv_[:, (n * 2 + wh) * 128:(n * 2 + wh + 1) * 128]
                                        tpv = P.op("tensor", lambda e, o_=o_, lhs=lhs, pTb=pTb, c_=c_, m=m, pc=pc: e.matmul(
                                            o_, lhs, pTb[:, c_:c_ + 128], start=(m == 0), stop=(m == len(pc) - 1)),
                                            deps=[tmk, tv, vrel2], sig=(n == 1 and wh == 1 and m == len(pc) - 1))
                            pTring.release(pi2, [tpv])
                            tev_l = []
                            for n, (tau, qsl, pc) in enumerate(layout):
                                src = pv_[:, n * 256:(n + 1) * 256].rearrange("p (a b) -> p a b", a=2)
                                dst = ND[:, :, qsl]
                                if g == 0:
                                    tev_l.append(P.op("vector", lambda e, dst=dst, src=src: e.tensor_copy(dst, src), deps=[tpv, nd_rel]))
                                else:
                                    tev_l.append(P.op("vector", lambda e, dst=dst, src=src: e.tensor_tensor(dst, src, dst, ALU.add), deps=[tpv]))
                            pvring.release(vi2, tev_l)
                            return tpv, tev_l

                        SK = 2
                        ctxs = {}
                        for step in range(8 + SK):
                            if step < 8:
                                ctxs[step] = emit_front(step)
                            if step - SK >= 0:
                                last_pe, tev = emit_back(ctxs.pop(step - SK))
                        qkring.release(qi, [last_pe])
                        vring.release(vi, [last_pe])
                        if l == 0 and hi_ == 0:
                            dbg_dump("qk", qkb[:])
                            dbg_dump("vv", vb[:])
                            dbg_dump("ND", ND[:])
                            if "qk" in dbg_d:
                                for _k in list(qkring.rel): pass
                                qkring.rel = [P.barrier()]
                                vring.rel = [P.barrier()]
                        if g == 2:
                            tf = P.op("vector", lambda e: e.reciprocal(ND[:, 1, :], ND[:, 1, :]), deps=[tev])
                            tf = P.op("vector", lambda e, j=j: e.tensor_tensor(oAT[:, j, :], ND[:, 0, :], ND[:, 1, :], ALU.mult), deps=[tf])
                            nd_rel = [tf]
                    else:
                        h = a
                        if False:
                            tail = P.barrier()
                            zring.rel = [list(tail) for _ in range(3)]
                            oring.rel = [list(tail) for _ in range(2)]
                        def b_prepare(pend):
                            wi_, wbuf_, wtoks_ = pend
                            qi_, qkb_, qrel_ = qkring.get()
                            vi_, vb_, vrel_ = vring.get()
                            stt = dict(wi=wi_, qi=qi_, vi=vi_, qkb=qkb_, vb=vb_, tq=[], tk=[], tv=[], lastv=None)
                            pieces = []
                            for tb in range(4):
                                pieces.append(lambda tb=tb: stt["tq"].extend(
                                    inproj_fm(wbuf_[:, 0], qkb_[:, 0, :], SCALE, "vector", bring, [wtoks_, qrel_], None, tbs=[tb])))
                            for tb in range(4):
                                pieces.append(lambda tb=tb: stt["tk"].extend(
                                    inproj_fm(wbuf_[:, 1], qkb_[:, 1, :], None, "vector", bring, [wtoks_, qrel_], None, tbs=[tb])))

                            def vpiece(t4):
                                bi, bk, brel = bring.get()
                                pb_ = pbk[bk]
                                tm = None
                                for tq4 in range(4):
                                    tau = t4 * 4 + tq4
                                    for kc in range(KC):
                                        tm = P.op("tensor", lambda e, pb_=pb_, tq4=tq4, kc=kc, tau=tau: e.matmul(
                                            pb_[:, tq4 * 128:(tq4 + 1) * 128], xT[:, kc, tau * 128:(tau + 1) * 128], wbuf_[:, 2, kc, :],
                                            start=(kc == 0), stop=(kc == KC - 1)),
                                            deps=[wtoks_, brel], sig=(tq4 == 3 and kc == KC - 1))
                                dv = vb_[:, t4 * 4:(t4 + 1) * 4, :]
                                sv = pb_.rearrange("p (a b) -> p a b", a=4)
                                te = P.op("vector", lambda e, dv=dv, sv=sv: e.tensor_copy(dv, sv), deps=[tm, vrel_])
                                bring.release(bi, [te])
                                stt["tv"].append(te)
                                stt["lastv"] = tm
                            for t4 in range(4):
                                pieces.append(lambda t4=t4: vpiece(t4))
                            stt["pieces"] = pieces
                            return stt

                        def run_pieces(stt, n=None):
                            k_ = 0
                            while stt["pieces"] and (n is None or k_ < n):
                                stt["pieces"].pop(0)()
                                k_ += 1
                            if not stt["pieces"] and not stt.get("released"):
                                wring.release(stt["wi"], [stt["lastv"]])
                                stt["released"] = True

                        if bstate.get("cur") is None:
                            cur = b_prepare((wi, wbuf, wtoks))
                            run_pieces(cur)
                        else:
                            cur = bstate["cur"]
                            run_pieces(cur)
                        nxt = b_prepare(pending) if hi_ + 1 < len(heads) else None
                        bstate["cur"] = nxt
                        qi, vi, qkb, vb = cur["qi"], cur["vi"], cur["qkb"], cur["vb"]
                        qT = qkb[:, 0, :]
                        kT = qkb[:, 1, :]
                        tq, tk_, tv = cur["tq"], cur["tk"], cur["tv"]
                        items = []
                        for qc in range(4):
                            kbs = list(range(4 * qc + 3, -1, -1))
                            for idx, kb in enumerate(kbs):
                                c0 = max(128 * kb - 512 * qc, 0)
                                items.append(dict(qc=qc, kb=kb, idx=idx, c0=c0, diag=(kb >= 4 * qc), last=(idx == len(kbs) - 1)))
                        n_it = len(items)
                        st = {"crel": {}, "sprel": {}, "arel": {}, "obank": None}
                        last_tokens = []

                        def stageA(s):
                            it = items[s]
                            zi, zbk, zrel = zring.get()
                            it["zi"], it["z"] = zi, pbk[zbk]
                            c0, qc, kb = it["c0"], it["qc"], it["kb"]
                            z = it["z"]
                            it["tA"] = P.op("tensor", lambda e, kT=kT, qT=qT: e.matmul(
                                z[:, c0:512], kT[:, kb * 128:(kb + 1) * 128], qT[:, qc * 512 + c0:(qc + 1) * 512],
                                start=True, stop=True), deps=[tq, tk_, zrel])

                        def stageB(s):
                            it = items[s]
                            c0, z = it["c0"], it["z"]
                            ei, eb, erel = ering.get()
                            spx = spb[s % 3]
                            it["sp"] = spx
                            t1_ = P.op("scalar", lambda e: e.activation(eb[:, c0:512], z[:, c0:512], AF.Exp), deps=[it["tA"], erel])
                            t2_ = P.op("scalar", lambda e: e.activation(spx[:, c0:512], eb[:, c0:512], AF.Ln, bias=1.0),
                                       deps=[t1_, st["sprel"].get(s % 3)])
                            ering.release(ei, [t2_])
                            if it["diag"]:
                                t2_ = P.op("gpsimd", lambda e: e.tensor_tensor(
                                    spx[:, c0:c0 + 128], spx[:, c0:c0 + 128], MSTRICT, ALU.mult), deps=[t2_] + cdeps)
                            it["tB"] = t2_
                            cn = cbuf[s % 3]
                            it["C"] = cn
                            crel = st["crel"].get(s % 3)
                            if it["idx"] == 0:
                                it["tC9"] = P.op("vector", lambda e: e.tensor_copy(cn[:, c0:512], spx[:, c0:512]), deps=[t2_, crel])
                            else:
                                pit = items[s - 1]
                                c1 = pit["c0"]
                                cp = pit["C"]
                                tt_ = P.op("vector", lambda e: e.tensor_tensor(cn[:, c1:512], cp[:, c1:512], spx[:, c1:512], ALU.add),
                                           deps=[t2_, pit["tC9"], crel])
                                if c0 < c1:
                                    tt_ = P.op("vector", lambda e: e.tensor_copy(cn[:, c0:c1], spx[:, c0:c1]), deps=[t2_, crel, tt_])
                                it["tC9"] = tt_

                        def stageC(s):
                            it = items[s]
                            c0, z, spx = it["c0"], it["z"], it["sp"]
                            tc = P.op("tensor", lambda e: e.matmul(z[:, c0:512], NEGU, spx[:, c0:512], start=False, stop=True,
                                                                   skip_group_check=True), deps=[it["tB"]] + cdeps, sig=(it["idx"] == 0))
                            if it["idx"] > 0:
                                pit = items[s - 1]
                                c1, cp = pit["c0"], pit["C"]
                                tc = P.op("tensor", lambda e: e.matmul(z[:, c1:512], NEGONES, cp[:, c1:512], start=False, stop=True,
                                                                       skip_group_check=True), deps=[pit["tC9"]])
                            it["tC"] = tc
                            st["sprel"][s % 3] = [tc, it["tC9"]]
                            if s >= 1:
                                st["crel"][(s - 1) % 3] = [tc]

                        def stageD(s):
                            it = items[s]
                            c0, z = it["c0"], it["z"]
                            ab = abuf[s % 3]
                            it["A"] = ab
                            td = P.op("scalar", lambda e: e.activation(ab[:, c0:512], z[:, c0:512], AF.Exp),
                                      deps=[it["tC"], st["arel"].get(s % 3)])
                            zring.release(it["zi"], [td])
                            if it["diag"]:
                                td = P.op("gpsimd", lambda e: e.tensor_tensor(
                                    ab[:, c0:c0 + 128], ab[:, c0:c0 + 128], MSTRICT, ALU.mult), deps=[td] + cdeps)
                            it["tD"] = td

                        def stageE(s):
                            it = items[s]
                            c0, ab, kb, qc = it["c0"], it["A"], it["kb"], it["qc"]
                            if it["idx"] == 0:
                                oi, obk, orel = oring.get()
                                st["obank"] = (oi, pbk[obk], orel)
                            oi, ob, orel = st["obank"]
                            te_ = P.op("tensor", lambda e, vb=vb: e.matmul(ob[:, c0:512], vb[:, kb, :], ab[:, c0:512],
                                                                    start=(it["idx"] == 0), stop=it["last"], skip_group_check=True),
                                       deps=[it["tD"], tv, orel])
                            st["arel"][s % 3] = [te_]
                            if it["last"]:
                                tev_ = P.op("vector", lambda e, h=h: e.tensor_copy(oBT[:, h, qc * 512:(qc + 1) * 512], ob), deps=[te_])
                                oring.release(oi, [tev_])
                                last_tokens.append(te_)

                        for s in range(n_it + 2):
                            if s < n_it:
                                stageA(s)
                                stageB(s)
                            if 0 <= s - 1 < n_it:
                                stageC(s - 1)
                                stageD(s - 1)
                            if 0 <= s - 2 < n_it:
                                stageE(s - 2)
                            if nxt is not None and s % 3 == 2:
                                run_pieces(nxt, 1)
                        if nxt is not None:
                            run_pieces(nxt)
                        qkring.release(qi, [last_tokens[-1]])
                        vring.release(vi, [last_tokens[-1]])
                if l == 0:
                    dbg_dump("oAT", oAT[:], None)
                    dbg_dump("oBT", oBT[:], None)
        if stop_after == "attn":
            return

        with P.phase():
            wg = [P.sbuf(f"wg{i}", [128, 2, KC, 128], BF16) for i in range(2)]
            wgring = Ring(wg)
            wpa = [P.sbuf(f"wpa{i}", [128, 4, 128], BF16) for i in range(2)]
            wpb = [P.sbuf(f"wpb{i}", [128, 8, 128], BF16) for i in range(2)]
            sg = [P.sbuf(f"sg{i}", [128, 2, 512], F32) for i in range(2)]
            sgring = Ring(sg)
            mm = [P.sbuf(f"mm{i}", [128, 2, 512], F32) for i in range(2)]
            mmring = Ring(mm)
            mo = [P.sbuf(f"mo{i}", [128, 512], BF16) for i in range(2)]
            moring = Ring(mo)
            bset = Ring([(0, 1, 2, 3), (4, 5, 6, 7)])

            def load_c(c):
                wi, wb, wrel = wgring.get()
                toks = [P.dma("gpsimd", wb[:, 0], win_d[l, 60 + c], deps=wrel, stream=f"wg{wi}"),
                        P.dma("gpsimd", wb[:, 1], win_d[l, 76 + c], deps=wrel, stream=f"wg{wi}"),
                        P.dma("gpsimd", wpa[wi][:], pa_d[l, c], deps=wrel, stream=f"wg{wi}"),
                        P.dma("gpsimd", wpb[wi][:], pb_d[l, c], deps=wrel, stream=f"wg{wi}")]
                return wi, wb, toks

            pend = load_c(0)
            for c in range(16):
                wi, wb, wtoks = pend
                if c + 1 < 16:
                    pend = load_c(c + 1)
                tlast = None
                for tb in range(4):
                    bi, banks, brel = bset.get()
                    tsl = slice(tb * 512, (tb + 1) * 512)
                    tms = []
                    for n in range(2):
                        pb_ = pbk[banks[n]]
                        for kc in range(KC):
                            tm = P.op("tensor", lambda e, pb_=pb_, n=n, kc=kc, tsl=tsl, wb=wb: e.matmul(
                                pb_, wb[:, n, kc, :], xT[:, kc, tsl], start=(kc == 0), stop=(kc == KC - 1)),
                                deps=[wtoks, brel], sig=(kc == KC - 1))
                        tms.append(tm)
                    pb_ = pbk[banks[2]]
                    for kc in range(4):
                        tm = P.op("tensor", lambda e, pb_=pb_, kc=kc, tsl=tsl, wi=wi: e.matmul(
                            pb_, wpa[wi][:, kc, :], oAT[:, kc, tsl], start=(kc == 0), stop=(kc == 3)),
                            deps=[wtoks, brel], sig=(kc == 3))
                    tms.append(tm)
                    pb_ = pbk[banks[3]]
                    for kc in range(8):
                        tm = P.op("tensor", lambda e, pb_=pb_, kc=kc, tsl=tsl, wi=wi: e.matmul(
                            pb_, wpb[wi][:, kc, :], oBT[:, kc, tsl], start=(kc == 0), stop=(kc == 7)),
                            deps=[wtoks, brel], sig=(kc == 7))
                    tms.append(tm)
                    tlast = tm
                    si, sgb, srel = sgring.get()
                    ta = P.op("scalar", lambda e, sgb=sgb, b0=pbk[banks[0]]: e.activation(sgb[:, 0, :], b0, AF.Sigmoid), deps=[tms[0], srel])
                    tb_ = P.op("scalar", lambda e, sgb=sgb, b1=pbk[banks[1]]: e.activation(sgb[:, 1, :], b1, AF.Sigmoid), deps=[tms[1], srel])
                    mi, mmb, mrel = mmring.get()
                    tc = P.op("vector", lambda e, mmb=mmb, sgb=sgb, b2=pbk[banks[2]]: e.tensor_tensor(mmb[:, 0, :], sgb[:, 0, :], b2, ALU.mult), deps=[ta, tms[2], mrel])
                    td = P.op("vector", lambda e, mmb=mmb, sgb=sgb, b3=pbk[banks[3]]: e.tensor_tensor(mmb[:, 1, :], sgb[:, 1, :], b3, ALU.mult), deps=[tb_, tms[3], mrel])
                    bset.release(bi, [ta, tb_, tc, td])
                    sgring.release(si, [tc, td])
                    oi, mob, orel = moring.get()
                    te = P.op("gpsimd", lambda e, mob=mob, mmb=mmb: e.tensor_tensor(mob[:], mmb[:, 0, :], mmb[:, 1, :], ALU.add), deps=[tc, td, orel])
                    mmring.release(mi, [te])
                    tdm = P.dma("sync", mT_d[tb * 4:(tb + 1) * 4, :, c, :].rearrange("t p n -> p t n"),
                                mob[:].rearrange("p (t n) -> p t n", t=4), deps=[te], stream=f"mo{oi}")
                    moring.release(oi, [tdm])
                wgring.release(wi, [tlast])
            if l == 0:
                dbg_dump("mT", mT_d)

    def phase_mixer_out(l, res_src_d, res_dst_d):
        with P.phase():
            wo = P.sbuf("wo", [128, KC, D], BF16)
            two = [P.dma("gpsimd", wo[:, :, nb4 * 512:(nb4 + 1) * 512], wo_d[l, :, :, nb4 * 512:(nb4 + 1) * 512], stream=f"wo{nb4}") for nb4 in range(4)]
            mt = [P.sbuf(f"mt{i}", [128, KC, 128], BF16) for i in range(2)]
            mtring = Ring(mt)
            yring = Ring([0, 1])
            ln = LNEpilogue(l, 0, res_src_d, res_dst_d)
            mtpf = {}

            def mt_prefetch(tt):
                mi, mtb, mrel = mtring.get()
                tl = P.dma("sync", mtb[:], mT_d[tt], deps=mrel, stream=f"mt{mi}")
                mtpf[tt] = (mi, mtb, tl)

            mt_prefetch(0)
            ln.prefetch(0)
            for tt in range(NT):
                if tt + 1 < NT:
                    mt_prefetch(tt + 1)
                    ln.prefetch(tt + 1)
                mi, mtb, tl = mtpf.pop(tt)
                yi, yh, yrel = yring.get()
                yp = pall[:, yh * 2048:(yh + 1) * 2048]
                tm = None
                for nb_ in range(4):
                    for kc in range(KC):
                        tm = P.op("tensor", lambda e, yp=yp, nb_=nb_, kc=kc, mtb=mtb: e.matmul(
                            yp[:, nb_ * 512:(nb_ + 1) * 512], mtb[:, kc, :], wo[:, kc, nb_ * 512:(nb_ + 1) * 512],
                            start=(kc == 0), stop=(kc == KC - 1)), deps=[tl, two[nb_], yrel], sig=(nb_ == 3 and kc == KC - 1))
                mtring.release(mi, [tm])
                tz = ln.run(tt, yp, [tm])
                yring.release(yi, [tz])
            ln.done()

    class LNEpilogue:
        def __init__(self, l, which, res_src_d, res_dst_d, store_q="scalar"):
            self.res_src_d, self.res_dst_d = res_src_d, res_dst_d
            self.store_q = store_q
            self.gb = P.sbuf("ln_gbuf", [128, 2, D], F32)
            self.tg = [P.dma("sync", self.gb[:, 0, :], lng_d[l, which, 0], stream="lng"),
                       P.dma("sync", self.gb[:, 1, :], lng_d[l, which, 1], stream="lng")]
            self.hr = [P.sbuf(f"hr{i}", [128, D], F32) for i in range(2)]
            self.hring = Ring(self.hr)
            self.zb = [P.sbuf(f"zb{i}", [128, D], F32) for i in range(3)]
            self.zring = Ring(self.zb)
            self.stt = [P.sbuf(f"lnst{i}", [128, 40], F32) for i in range(3)]
            self.sring = Ring(self.stt)
            self.toks = []
            self.pf = {}

        def prefetch(self, tt):
            hi, hb, hrel = self.hring.get()
            tl = P.dma("sync", hb[:], self.res_src_d[tt * 128:(tt + 1) * 128, :], deps=hrel, stream=f"hr{hi}")
            self.pf[tt] = (hi, hb, tl)

        def run(self, tt, y_ap, ydeps, y_is_psum=True):
            if tt not in self.pf:
                self.prefetch(tt)
            hi, hb, tl = self.pf.pop(tt)
            zi, zb, zrel = self.zring.get()
            si, stt, srel = self.sring.get()
            tz = P.op("vector", lambda e: e.scalar_tensor_tensor(zb[:], hb[:], ALPHA, y_ap, ALU.mult, ALU.add), deps=[tl, ydeps, zrel])
            self.hring.release(hi, [tz])
            ts = None
            for c in range(4):
                ts = P.op("vector", lambda e, c=c: e.bn_stats(stt[:, c * 6:(c + 1) * 6], zb[:, c * 512:(c + 1) * 512]), deps=[tz, srel])
            mv = stt[:, 24:26]
            ts = P.op("vector", lambda e: e.bn_aggr(mv, stt[:, 0:24]), deps=[ts])
            sd = stt[:, 26:27]
            rstd = stt[:, 27:28]
            nmr = stt[:, 28:29]
            ta = P.op("scalar", lambda e: e.activation(sd, stt[:, 25:26], AF.Sqrt, bias=LN_EPS, scale=1.0), deps=[ts])
            tb_ = P.op("vector", lambda e: e.reciprocal(rstd, sd), deps=[ta])
            tb_ = P.op("vector", lambda e: e.scalar_tensor_tensor(nmr, stt[:, 24:25], -1.0, rstd, ALU.mult, ALU.mult), deps=[tb_])
            tn = P.op("scalar", lambda e: e.activation(zb[:], zb[:], AF.Identity, bias=nmr, scale=rstd), deps=[tb_])
            tg_ = P.op("vector", lambda e: e.tensor_tensor(zb[:], zb[:], self.gb[:, 0, :], ALU.mult), deps=[tn, self.tg])
            tb2 = P.op("vector", lambda e: e.tensor_tensor(zb[:], zb[:], self.gb[:, 1, :], ALU.add), deps=[tg_])
            self.sring.release(si, [tb2])
            td = P.dma(self.store_q, self.res_dst_d[tt * 128:(tt + 1) * 128, :], zb[:], deps=[tb2], stream=f"zb{zi}")
            self.zring.release(zi, [td])
            self.toks.append(td)
            return tz

        def done(self):
            pass

    def phase_moe(l, res_src_d, res_dst_d, final):
        yacc = P.sbuf("yacc", [128, 8, D], F32)
        ln_toks = []
        for half in range(2):
            with P.phase():
                wu = [P.sbuf(f"wu{i}", [128, KC, 2, 128], BF16) for i in range(2)]
                wuring = Ring(wu)
                wd = [P.sbuf(f"wd{i}", [128, 4, 1024], BF16) for i in range(2)]
                wdring = Ring(wd)
                act = P.sbuf("act", [128, 4, 1024], BF16)
                sl_ = [P.sbuf(f"silu{i}", [128, 512], F32) for i in range(2)]
                slring = Ring(sl_)
                upring = Ring([(0, 1), (2, 3)])
                dnring = Ring([2, 3])
                act_rel = []
                jobs = []
                for e_ in range(16):
                    for i in range(4):
                        jobs.append(("u", e_, i))
                    for d_ in range(2):
                        jobs.append(("d", e_, d_))
                loaded = {}

                def issue(job):
                    kind, e_, k = job
                    if kind == "u":
                        wi, wb, wrel = wuring.get()
                        t = P.dma("gpsimd", wb[:], wup_d[l, e_, k], deps=wrel, stream=f"wu{wi}")
                        loaded[job] = (wuring, wi, wb, [t])
                    else:
                        wi, wb, wrel = wdring.get()
                        t = P.dma("gpsimd", wb[:], wdn_d[l, e_, :, k * 1024:(k + 1) * 1024].rearrange("(c p) n -> p c n", p=128),
                                  deps=wrel, stream=f"wd{wi}")
                        loaded[job] = (wdring, wi, wb, [t])

                PF = 2
                nu = 0
                for jn in range(min(PF, len(jobs))):
                    issue(jobs[jn])
                nxt = min(PF, len(jobs))
                act_w = []
                dn_last = []
                for jn, job in enumerate(jobs):
                    kind, e_, k = job
                    ring_, wi, wb, wtoks = loaded.pop(job)
                    if kind == "u":
                        i = k
                        tlast = None
                        for tbh in range(2):
                            tsl = slice(half * 1024 + tbh * 512, half * 1024 + (tbh + 1) * 512)
                            ui, banks, urel = upring.get()
                            tms = []
                            for n in range(2):
                                pb_ = pbk[banks[n]]
                                for kc in range(KC):
                                    tm = P.op("tensor", lambda e, pb_=pb_, n=n, kc=kc, tsl=tsl, wb=wb: e.matmul(
                                        pb_, wb[:, kc, n, :], xT[:, kc, tsl], start=(kc == 0), stop=(kc == KC - 1)),
                                        deps=[wtoks, urel], sig=(kc == KC - 1))
                                tms.append(tm)
                            tlast = tm
                            si, sb_, srel = slring.get()
                            ta = P.op("scalar", lambda e, sb_=sb_, b0=pbk[banks[0]]: e.activation(sb_[:], b0, AF.Silu), deps=[tms[0], srel])
                            av = act[:, i, tbh * 512:(tbh + 1) * 512]
                            tb_ = P.op("vector", lambda e, av=av, sb_=sb_, b1=pbk[banks[1]]: e.tensor_tensor(av, sb_[:], b1, ALU.mult),
                                       deps=[ta, tms[1], dn_last])
                            slring.release(si, [tb_])
                            upring.release(ui, [ta, tb_])
                            act_w.append(tb_)
                        ring_.release(wi, [tlast])
                    else:
                        d_ = k
                        tlast = None
                        tevs = []
                        for t8 in range(8):
                            di, dh, drel = dnring.get()
                            yp = pall[:, dh * 1024:(dh + 1) * 1024]
                            tm = None
                            for nb_ in range(2):
                                for c in range(4):
                                    tm = P.op("tensor", lambda e, yp=yp, nb_=nb_, c=c, t8=t8, wb=wb: e.matmul(
                                        yp[:, nb_ * 512:(nb_ + 1) * 512], act[:, c, t8 * 128:(t8 + 1) * 128],
                                        wb[:, c, nb_ * 512:(nb_ + 1) * 512], start=(c == 0), stop=(c == 3)),
                                        deps=[wtoks, act_w, drel], sig=(nb_ == 1 and c == 3))
                            tlast = tm
                            ya = yacc[:, t8, d_ * 1024:(d_ + 1) * 1024]
                            gsc = gates[:, half * 8 + t8, e_:e_ + 1]
                            if e_ == 0:
                                tev = P.op("vector", lambda e, ya=ya, yp=yp, gsc=gsc: e.tensor_scalar(ya, yp, gsc, None, ALU.mult), deps=[tm, ln_toks])
                            else:
                                tev = P.op("vector", lambda e, ya=ya, yp=yp, gsc=gsc: e.scalar_tensor_tensor(ya, yp, gsc, ya, ALU.mult, ALU.add), deps=[tm])
                            dnring.release(di, [tev])
                            tevs.append(tev)
                        ring_.release(wi, [tlast])
                        if d_ == 1:
                            dn_last = [tlast]
                            act_w = []
                    if nxt < len(jobs):
                        issue(jobs[nxt])
                        nxt += 1
            with P.phase():
                ln = LNEpilogue(l, 1, res_src_d, res_dst_d)
                ln.prefetch(half * 8)
                for t8 in range(8):
                    if t8 + 1 < 8:
                        ln.prefetch(half * 8 + t8 + 1)
                    tz = ln.run(half * 8 + t8, yacc[:, t8, :], [])
                    ln_toks.append(tz)
                if final:
                    out_toks.extend(ln.toks)


    def phase_moe_sparse(l, res_src_d, res_dst_d, final):
        V = "vector"
        IOA = bass.IndirectOffsetOnAxis
        with P.phase():
            SHI = P.sbuf("SHI", [128, NT], I32)
            SLO = P.sbuf("SLO", [128, NT], I32)
            GHI = P.sbuf("GHI", [128, NT], F32)
            GLO = P.sbuf("GLO", [128, NT], F32)
            IDXU = P.sbuf("IDXU", [128, KT * 8], I32)
            IDXD = P.sbuf("IDXD", [128, KT * 4], I32)
            with P.phase():
                def W(name, n, dt=F32):
                    return P.sbuf("r_" + name, [128, n], dt)

                def v3(t, a):
                    return t[:].rearrange("p (a b) -> p a b", a=a)

                gfl = gates[:].rearrange("p t e -> p (t e)")
                Mf = selmask[:].rearrange("p t e -> p (t e)")
                Mb = W("Mb", NT * 16, BF16)
                c0 = P.op(V, lambda e: e.tensor_copy(Mb[:], Mf))
                rk = pbk[0]
                cn = pbk[1]
                tm = None
                for tt in range(NT):
                    tm = P.op("tensor", lambda e, tt=tt: e.matmul(rk[:, tt * 16:(tt + 1) * 16], MSTRICT, Mb[:, tt * 16:(tt + 1) * 16],
                                                                start=True, stop=(tt == 0)), deps=[c0] + cdeps, sig=False)
                    for t2 in range(tt):
                        tm = P.op("tensor", lambda e, tt=tt, t2=t2: e.matmul(rk[:, tt * 16:(tt + 1) * 16], ONES, Mb[:, t2 * 16:(t2 + 1) * 16],
                                                                         start=False, stop=(t2 == tt - 1)), sig=False)
                for tt in range(NT):
                    tm = P.op("tensor", lambda e, tt=tt: e.matmul(cn[:, 0:16], ONES, Mb[:, tt * 16:(tt + 1) * 16],
                                                                start=(tt == 0), stop=(tt == NT - 1)), sig=(tt == NT - 1))
                nf, tmpf, ptf, ts, ends, base1 = W("nf", 16), W("tmpf", 16), W("ptf", 16), W("ts", 16), W("ends", 16), W("base1", 16)
                pti = W("pti", 16, I32)
                c = P.op(V, lambda e: e.tensor_scalar(tmpf[:], cn[:, 0:16], 255.0, None, ALU.add), deps=[tm])
                c = P.op(V, lambda e: e.tensor_copy(pti[:], tmpf[:]), deps=[c])
                c = P.op(V, lambda e: e.tensor_scalar(pti[:], pti[:], 8, None, ALU.arith_shift_right), deps=[c])
                c = P.op(V, lambda e: e.tensor_copy(ptf[:], pti[:]), deps=[c])
                c = P.op(V, lambda e: e.tensor_scalar(ts[:, 0:1], ptf[:, 0:1], 0.0, None, ALU.mult), deps=[c])
                for e_ in range(1, 16):
                    c = P.op(V, lambda e, e_=e_: e.tensor_tensor(ts[:, e_:e_ + 1], ts[:, e_ - 1:e_], ptf[:, e_ - 1:e_], ALU.add), deps=[c])
                c = P.op(V, lambda e: e.tensor_tensor(ends[:], ts[:], ptf[:], ALU.add), deps=[c])
                c = P.op(V, lambda e: e.tensor_scalar(base1[:], ts[:], float(TS), 1.0, ALU.mult, ALU.add), deps=[c])
                SL, A_, eq = W("SL", 256), W("A", 256), W("eq", 256)
                amax, asum, tf1, tf2, gsum = W("amax", 16), W("asum", 16), W("tf1", 16), W("tf2", 16), W("gsum", 16)
                b1b = base1[:].rearrange("p (o e) -> p o e", o=1).broadcast_to([128, NT, 16])
                c = P.op(V, lambda e: e.tensor_tensor(v3(SL, NT), rk[:, 0:256].rearrange("p (a b) -> p a b", a=NT), b1b, ALU.add), deps=[c])
                c = P.op(V, lambda e: e.tensor_tensor(A_[:], SL[:], Mf, ALU.mult), deps=[c])
                c = P.op(V, lambda e: e.reduce_max(amax[:], v3(A_, NT), AX.X), deps=[c])
                c = P.op(V, lambda e: e.reduce_sum(asum[:], v3(A_, NT), AX.X), deps=[c])
                c = P.op(V, lambda e: e.tensor_scalar(tf1[:], amax[:], -1.0, None, ALU.add), deps=[c])
                c = P.op(V, lambda e: e.tensor_copy(SHI[:], tf1[:]), deps=[c])
                c = P.op(V, lambda e: e.tensor_tensor(tf2[:], asum[:], amax[:], ALU.subtract), deps=[c])
                c = P.op(V, lambda e: e.tensor_scalar(tf2[:], tf2[:], -1.0, None, ALU.add), deps=[c])
                c = P.op(V, lambda e: e.tensor_copy(SLO[:], tf2[:]), deps=[c])
                amb = amax[:].rearrange("p (t o) -> p t o", o=1).broadcast_to([128, NT, 16])
                c = P.op(V, lambda e: e.tensor_tensor(v3(eq, NT), v3(A_, NT), amb, ALU.is_equal), deps=[c])
                c = P.op(V, lambda e: e.tensor_tensor(eq[:], eq[:], gfl, ALU.mult), deps=[c])
                c = P.op(V, lambda e: e.reduce_sum(GHI[:], v3(eq, NT), AX.X), deps=[c])
                c = P.op(V, lambda e: e.reduce_sum(gsum[:], gates[:], AX.X), deps=[c])
                c = P.op(V, lambda e: e.tensor_tensor(GLO[:], gsum[:], GHI[:], ALU.subtract), deps=[c])
                cmp_, ek, eku = W("cmp", KT * 16), W("ek", KT), W("eku", KT)
                enb = ends[:].rearrange("p (o e) -> p o e", o=1).broadcast_to([128, KT, 16])
                kgb = cscal[:, 8:8 + KT].rearrange("p (k o) -> p k o", o=1).broadcast_to([128, KT, 16])
                c = P.op(V, lambda e: e.tensor_tensor(v3(cmp_, KT), enb, kgb, ALU.is_le), deps=[c] + cdeps)
                c = P.op(V, lambda e: e.reduce_sum(ek[:], v3(cmp_, KT), AX.X), deps=[c])
                un = W("un", KT)
                c = P.op(V, lambda e: e.tensor_scalar(un[:], ek[:], 15.5, 1.0e6, ALU.is_gt, ALU.mult), deps=[c])
                c = P.op(V, lambda e: e.tensor_scalar(ek[:], ek[:], 15.0, None, ALU.min), deps=[c])
                iuf, idf = W("iuf", KT * 8), W("idf", KT * 4)
                c = P.op(V, lambda e: e.tensor_scalar(eku[:], ek[:], 1024.0, float(l * 16384), ALU.mult, ALU.add), deps=[c])
                c = P.op(V, lambda e: e.tensor_tensor(eku[:], eku[:], un[:], ALU.add), deps=[c])
                ekb8 = eku[:].rearrange("p (k o) -> p k o", o=1).broadcast_to([128, KT, 8])
                pj8 = cscal[:, 40:48].rearrange("p (o j) -> p o j", o=1).broadcast_to([128, KT, 8])
                c = P.op(V, lambda e: e.tensor_tensor(v3(iuf, KT), ekb8, pj8, ALU.add), deps=[c])
                c = P.op(V, lambda e: e.tensor_copy(IDXU[:], iuf[:]), deps=[c])
                c = P.op(V, lambda e: e.tensor_scalar(eku[:], ek[:], 512.0, float(l * 8192), ALU.mult, ALU.add), deps=[c])
                c = P.op(V, lambda e: e.tensor_tensor(eku[:], eku[:], un[:], ALU.add), deps=[c])
                ekb4 = eku[:].rearrange("p (k o) -> p k o", o=1).broadcast_to([128, KT, 4])
                pj4 = cscal[:, 40:44].rearrange("p (o j) -> p o j", o=1).broadcast_to([128, KT, 4])
                c = P.op(V, lambda e: e.tensor_tensor(v3(idf, KT), ekb4, pj4, ALU.add), deps=[c])
                c = P.op(V, lambda e: e.tensor_copy(IDXD[:], idf[:]), deps=[c])
            if l == 0:
                dbg_dump("SHI", SHI[:])
                dbg_dump("SLO", SLO[:])
                dbg_dump("IDXU", IDXU[:])
            with P.phase():
                sxb = [P.sbuf(f"sx{i}", [128, D], F32) for i in range(4)]
                sring = Ring(sxb)
                sx16 = [P.sbuf(f"sxh{i}", [128, D], BF16) for i in range(4)]
                s16ring = Ring(sx16)
                for tt in range(NT):
                    i, buf, rel = sring.get()
                    tl = P.dma("sync", buf[:], res_src_d[tt * 128:(tt + 1) * 128, :], deps=rel, stream=f"sx{i}")
                    i2, b16, rel2 = s16ring.get()
                    if tt % 2 == 0:
                        tc_ = P.op("scalar", lambda e, b16=b16, buf=buf: e.copy(b16[:], buf[:]), deps=[tl, rel2])
                    else:
                        tc_ = P.op("vector", lambda e, b16=b16, buf=buf: e.tensor_copy(b16[:], buf[:]), deps=[tl, rel2])
                    sring.release(i, [tc_])
                    s1 = P.dma_fn("gpsimd", lambda e, b16=b16, tt=tt: e.indirect_dma_start(
                        out=XS_d[:, :], out_offset=IOA(ap=SHI[:, tt:tt + 1], axis=0), in_=b16[:, :], in_offset=None), deps=[tc_], stream=f"sc{i2}")
                    s2 = P.dma_fn("gpsimd", lambda e, b16=b16, tt=tt: e.indirect_dma_start(
                        out=XS_d[:, :], out_offset=IOA(ap=SLO[:, tt:tt + 1], axis=0), in_=b16[:, :], in_offset=None), deps=[tc_], stream=f"sc{i2}")
                    s16ring.release(i2, [s1, s2])
            with P.phase():
                wu3 = P.sbuf("swu3", [128, 8, 2048], BF16)
                wu = [xT[:, 0:8, :], xT[:, 8:16, :], wu3[:]]
                wd = [P.sbuf(f"swd{i}", [128, 4, 2048], BF16) for i in range(3)]
                wring = Ring([0, 1, 2])
                xtok = [P.sbuf(f"sxt{i}", [128, 2, D], BF16) for i in range(2)]
                xring = Ring(xtok)
                xsT = [P.sbuf(f"sxT{i}", [128, KC, TS], BF16) for i in range(2)]
                xTring = Ring(xsT)
                act = [P.sbuf(f"sact{i}", [128, 4, TS], BF16) for i in range(2)]
                aring = Ring(act)
                sl_ = [P.sbuf(f"ssl{i}", [128, TS], F32) for i in range(2)]
                slring = Ring(sl_)
                yst = [P.sbuf(f"syst{i}", [128, 1024], F32) for i in range(3)]
                yring = Ring(yst)
                upring = Ring([(0, 1), (2, 3)])
                dnring = Ring([2, 3])
                IDB = cbf[:, 896:1024]

                breg = {}

                def get_breg(e):
                    if "r" not in breg:
                        breg["r"] = e.to_reg(L * 16384 - 1)
                    return breg["r"]

                def gather_b(e, out_ap, in_ap, idx_ap):
                    reg = e.to_reg(L * 16384 - 1)
                    ins = e.indirect_dma_start(out=out_ap, out_offset=None, in_=in_ap,
                                               in_offset=IOA(ap=idx_ap, axis=0), bounds_check=reg, oob_is_err=False)
                    e.free_register(reg)
                    return ins

                def load_w(k):
                    wi, _, wrel = wring.get()
                    toks = []
                    for j in range(8):
                        toks.append(P.dma_fn("gpsimd", lambda e, wi=wi, j=j, k=k: gather_b(
                            e, wu[wi][:, j, :], wup_d.rearrange("l r d -> (l r) d"), IDXU[:, k * 8 + j:k * 8 + j + 1]), deps=wrel, stream=f"swu{wi}"))
                    for j in range(4):
                        toks.append(P.dma_fn("gpsimd", lambda e, wi=wi, j=j, k=k: gather_b(
                            e, wd[wi][:, j, :], wdn_d.rearrange("l r d -> (l r) d"), IDXD[:, k * 4 + j:k * 4 + j + 1]), deps=wrel, stream=f"swu{wi}"))
                    return wi, toks

                def load_x(k):
                    xi, xb, xrel = xring.get()
                    tx = P.dma("sync", xb[:], XS_d[k * TS:(k + 1) * TS, :].rearrange("(s p) d -> p s d", p=128), deps=[xrel], stream=f"sxt{xi}")
                    return xi, xb, tx

                def load_tile(k):
                    return load_w(k) + load_x(k)

                order = []
                lo_, hi_k = 0, KT - 1
                while lo_ <= hi_k:
                    order.append(lo_)
                    if hi_k != lo_:
                        order.append(hi_k)
                    lo_ += 1
                    hi_k -= 1
                wq = [load_w(order[0]), load_w(order[1])]
                xq = load_x(order[0])
                for kn, k in enumerate(order):
                    wi, wtoks = wq.pop(0)
                    xi, xb, tx = xq
                    if kn + 1 < KT:
                        xq = load_x(order[kn + 1])
                    if kn + 2 < KT:
                        wq.append(load_w(order[kn + 2]))
                    ti, xT_, trel = xTring.get()
                    tev = []
                    tlast = None
                    for half in range(4):
                        ui, banks, urel = upring.get()
                        bview = pbk[banks[0]].bitcast(BF16)
                        for q in range(8):
                            idx = half * 8 + q
                            sub, kc = idx // KC, idx % KC
                            tlast = P.op("tensor", lambda e, bview=bview, q=q, sub=sub, kc=kc, xb=xb: e.transpose(
                                bview[:, q * 128:(q + 1) * 128], xb[:, sub, kc * 128:(kc + 1) * 128], IDB),
                                deps=[tx, urel] + cdeps, sig=(q == 7))
                        sub0 = (half * 8) // KC
                        kc0 = (half * 8) % KC
                        dstv = xT_[:, kc0:kc0 + 8, sub0 * 128:(sub0 + 1) * 128]
                        srcv = bview.rearrange("p (a b) -> p a b", a=8)
                        if half % 2 == 0:
                            te = P.op("scalar", lambda e, dstv=dstv, srcv=srcv: e.copy(dstv, srcv), deps=[tlast, trel])
                        else:
                            te = P.op("vector", lambda e, dstv=dstv, srcv=srcv: e.tensor_copy(dstv, srcv), deps=[tlast, trel])
                        upring.release(ui, [te])
                        tev.append(te)
                    xring.release(xi, [tlast])
                    ai, ab_, arel = aring.get()
                    tms_last = None
                    acts = []
                    for i in range(4):
                        ui, banks, urel = upring.get()
                        tms = []
                        for n in range(2):
                            pb_ = pbk[banks[n]][:, 0:TS]
                            for kc in range(KC):
                                tm = P.op("tensor", lambda e, pb_=pb_, n=n, kc=kc, i=i, wi=wi, xT_=xT_: e.matmul(
                                    pb_, wu[wi][:, i * 2 + n, kc * 128:(kc + 1) * 128], xT_[:, kc, :],
                                    start=(kc == 0), stop=(kc == KC - 1)), deps=[wtoks, tev, urel], sig=(kc == KC - 1))
                            tms.append(tm)
                        tms_last = tm
                        si, sb_, srel = slring.get()
                        ta = P.op("scalar", lambda e, sb_=sb_, b0=pbk[banks[0]][:, 0:TS]: e.activation(sb_[:], b0, AF.Silu), deps=[tms[0], srel])
                        tb_ = P.op("vector", lambda e, ab_=ab_, i=i, sb_=sb_, b1=pbk[banks[1]][:, 0:TS]: e.tensor_tensor(ab_[:, i, :], sb_[:], b1, ALU.mult),
                                   deps=[ta, tms[1], arel])
                        slring.release(si, [tb_])
                        upring.release(ui, [ta, tb_])
                        acts.append(tb_)
                    xTring.release(ti, [tms_last])
                    tdl = None
                    for sub in range(2):
                        for d_ in range(2):
                            di, dh, drel = dnring.get()
                            yp = pall[:, dh * 1024:(dh + 1) * 1024]
                            for nb_ in range(2):
                                for c_ in range(4):
                                    tdl = P.op("tensor", lambda e, yp=yp, nb_=nb_, c_=c_, sub=sub, d_=d_, ab_=ab_, wi=wi: e.matmul(
                                        yp[:, nb_ * 512:(nb_ + 1) * 512], ab_[:, c_, sub * 128:(sub + 1) * 128],
                                        wd[wi][:, c_, d_ * 1024 + nb_ * 512:d_ * 1024 + (nb_ + 1) * 512], start=(c_ == 0), stop=(c_ == 3)),
                                        deps=[wtoks, acts, drel], sig=(nb_ == 1 and c_ == 3))
                            yi, yb, yrel = yring.get()
                            if (sub + d_) % 2 == 0:
                                tev_ = P.op("scalar", lambda e, yb=yb, yp=yp: e.copy(yb[:], yp), deps=[tdl, yrel])
                            else:
                                tev_ = P.op("vector", lambda e, yb=yb, yp=yp: e.tensor_copy(yb[:], yp), deps=[tdl, yrel])
                            dnring.release(di, [tev_])
                            tdo = P.dma("sync", YS_d[k * TS + sub * 128:k * TS + (sub + 1) * 128, d_ * 1024:(d_ + 1) * 1024], yb[:],
                                        deps=[tev_], stream=f"syst{yi}")
                            yring.release(yi, [tdo])
                    aring.release(ai, [tdl])
                    wring.release(wi, [tdl])
            with P.phase():
                yg = [P.sbuf(f"syg{i}", [128, 2, D], F32) for i in range(2)]
                gring = Ring(yg)
                ln = LNEpilogue(l, 1, res_src_d, res_dst_d, store_q="sync")

                def gath(tt):
                    gi, gb_, grel = gring.get()
                    t1_ = P.dma_fn("gpsimd", lambda e, gb_=gb_, tt=tt: e.indirect_dma_start(
                        out=gb_[:, 0, :], out_offset=None, in_=YS_d[:, :], in_offset=IOA(ap=SHI[:, tt:tt + 1], axis=0)), deps=grel, stream=f"syg{gi}")
                    t2_ = P.dma_fn("gpsimd", lambda e, gb_=gb_, tt=tt: e.indirect_dma_start(
                        out=gb_[:, 1, :], out_offset=None, in_=YS_d[:, :], in_offset=IOA(ap=SLO[:, tt:tt + 1], axis=0)), deps=grel, stream=f"syg{gi}")
                    return gi, gb_, [t1_, t2_]

                pg_ = gath(0)
                ln.prefetch(0)
                for tt in range(NT):
                    gi, gb_, gt = pg_
                    if tt + 1 < NT:
                        pg_ = gath(tt + 1)
                        ln.prefetch(tt + 1)
                    ta = P.op("scalar", lambda e, gb_=gb_, tt=tt: e.activation(gb_[:, 0, :], gb_[:, 0, :], AF.Identity, scale=GHI[:, tt:tt + 1]), deps=[gt])
                    tb_ = P.op(V, lambda e, gb_=gb_, tt=tt: e.scalar_tensor_tensor(gb_[:, 1, :], gb_[:, 1, :], GLO[:, tt:tt + 1], gb_[:, 0, :], ALU.mult, ALU.add), deps=[ta, gt])
                    tz = ln.run(tt, gb_[:, 1, :], [tb_])
                    gring.release(gi, [tz])
                if final:
                    out_toks.extend(ln.toks)

    def run_all():
        src = x_d
        for l in range(n_layers):
            phase_prep(src, l, router=False)
            if l == 0:
                dbg_dump("xT", xT[:])
            if stop_after == "prep":
                return
            with P.phase():
                phase_mixer(l)
            if stop_after in ("attn", "merge"):
                return
            phase_mixer_out(l, src, R_d[0] if l == 0 else R_d[2])
            src = R_d[0] if l == 0 else R_d[2]
            if l == 0:
                dbg_dump("h1", src)
            if stop_after == "ln1":
                return
            phase_prep(src, l, router=True)
            if l == 0:
                dbg_dump("gates", gates[:])
            if stop_after == "gates":
                return
            final = (l == n_layers - 1)
            dst = out_d if final else R_d[1]
            if SPARSE_MOE:
                phase_moe_sparse(l, src, dst, final)
            else:
                with P.phase():
                    phase_moe(l, src, dst, final)
            if stop_after in ("moe_y",):
                return
            src = dst

    run_all()
    if stop_after is not None and stop_after not in ("prep", "gates"):
        pass
    P.base = []
    P.finish_wait("sync", [out_toks, P.barrier()])
    P.emit()
    P.close()
    return nc


def make_consts():
    c = np.zeros((128, NCONST), np.float32)
    i = np.arange(128)
    key = i[:, None]
    qry = i[None, :]
    c[:, 0:128] = (key <= qry)
    c[:, 128:256] = (key >= qry)
    c[:, 256:384] = (key < qry)
    c[:, 384:512] = -1.0 * (key >= qry)
    c[:, 512:640] = -1.0
    c[:, 640:768] = 1.0
    perm = np.zeros((32, 32), np.float32)
    for m in range(32):
        perm[(m + 16) % 32, m] = 1.0
    c[0:32, 768:800] = perm
    c[:, 896:1024] = np.eye(128, dtype=np.float32)
    cs = np.zeros((128, 64), np.float32)
    cs[:, 8:40] = np.arange(32, dtype=np.float32)[None, :]
    cs[:, 40:48] = (np.arange(8)[None, :] * 128 + np.arange(128)[:, None]).astype(np.float32)
    half = 16
    inv_freq = (np.float32(500000.0) ** (np.float32(-2.0) * np.arange(half, dtype=np.float32) / np.float32(32))).astype(np.float32)
    cs[0:16, 0] = inv_freq
    cs[16:32, 0] = inv_freq
    cs[0:16, 1] = -1.0
    cs[16:32, 1] = 1.0
    return c, cs


def prep_inputs(x, positions, w_in, p_a, p_b, w_o, ln_g, ln_b, router_coarse, router_coarse_bias,
                router_fine, router_fine_bias, w_up, w_down):
    f = np.float32
    w_in_r = np.ascontiguousarray(np.asarray(w_in, f).reshape(L, KC, 128, 92, 128).transpose(0, 3, 2, 1, 4))
    pa_r = np.ascontiguousarray(np.asarray(p_a, f).reshape(L, 4, 128, 16, 128).transpose(0, 3, 2, 1, 4))
    pb_r = np.ascontiguousarray(np.asarray(p_b, f).reshape(L, 8, 128, 16, 128).transpose(0, 3, 2, 1, 4))
    wo_r = np.ascontiguousarray(np.asarray(w_o, f).reshape(L, KC, 128, D).transpose(0, 2, 1, 3))
    lng = np.stack([np.asarray(ln_g, f), np.asarray(ln_b, f)], axis=2)
    lng = np.ascontiguousarray(np.broadcast_to(lng[:, :, :, None, :], (L, 2, 2, 128, D)))
    wr = np.concatenate([np.asarray(router_coarse, f), np.asarray(router_fine, f)], axis=2)
    wr_r = np.ascontiguousarray(wr.reshape(L, KC, 128, 20).transpose(0, 2, 1, 3))
    rb = np.concatenate([np.asarray(router_coarse_bias, f), np.asarray(router_fine_bias, f)], axis=1)
    rb_r = np.ascontiguousarray(np.broadcast_to(rb[:, None, :], (L, 128, 20)))
    wu = np.asarray(w_up, f).reshape(L, 16, KC, 128, 2, 4, 128)
    wu_r = np.ascontiguousarray(wu.transpose(0, 1, 5, 4, 3, 2, 6)).reshape(L, 16 * 4 * 2 * 128, 2048)
    wd = np.ascontiguousarray(np.asarray(w_down, f)).reshape(L, 16 * 512, D)
    consts, cscal = make_consts()
    shared = {"w_in": w_in_r, "p_a": pa_r, "p_b": pb_r, "w_o": wo_r, "ln_gb": lng, "w_r": wr_r, "r_b": rb_r,
              "w_up": wu_r, "w_down": wd, "consts": consts, "cscal": cscal}
    x = np.asarray(x, f)
    positions = np.asarray(positions, np.int32)
    in_maps = []
    for b in range(NCORES):
        m = dict(shared)
        m["x"] = np.ascontiguousarray(x[b])
        m["pos"] = np.ascontiguousarray(np.broadcast_to(positions[b][None, :], (32, S)))
        in_maps.append(m)
    return in_maps


_NC_CACHE = {}


def kernel(x, positions, w_in, p_a, p_b, w_o, ln_g, ln_b, router_coarse, router_coarse_bias,
           router_fine, router_fine_bias, w_up, w_down):
    in_maps = prep_inputs(x, positions, w_in, p_a, p_b, w_o, ln_g, ln_b, router_coarse, router_coarse_bias,
                          router_fine, router_fine_bias, w_up, w_down)
    if "nc" not in _NC_CACHE:
        _NC_CACHE["nc"] = build_nc()
    nc = _NC_CACHE["nc"]
    res = run_bass_kernel_spmd(nc, in_maps, core_ids=list(range(NCORES)))
    out = np.stack([np.asarray(r["out"], np.float32) for r in res.results], axis=0)
    return out
```
